# Optimizing a Trainium2 kernel written in Bass

```python
import math
import jax
import jax.numpy as jnp
from jax import lax
import numpy as np

D_MODEL = 2048
BATCH = 2
SEQ = 8192
DEPTH = 1

HEAD_DIM = 128
N_Q_HEADS = D_MODEL // HEAD_DIM
N_KV_HEADS = N_Q_HEADS // 4
Q_BLOCK = 128
ROPE_THETA = 10000.0
ROPE_AXIS_DIM = HEAD_DIM // 2
GRID_W = 64
POOL_WIDTH = D_MODEL
POOL_WINDOWS = (2, 4, 8, 16)
N_POOL_GROUPS = len(POOL_WINDOWS)
POOL_GROUP_W = POOL_WIDTH // N_POOL_GROUPS
Q_W = N_Q_HEADS * HEAD_DIM
KV_W = N_KV_HEADS * HEAD_DIM
IN_SPLITS = (Q_W, Q_W + KV_W, Q_W + 2 * KV_W, Q_W + 2 * KV_W + POOL_WIDTH, Q_W + 2 * KV_W + POOL_WIDTH + D_MODEL)
IN_W = Q_W + 2 * KV_W + POOL_WIDTH + 2 * D_MODEL
N_EXPERTS = 64
TOP_K = 6
N_EXPERT_GROUPS = 8
TOPK_GROUPS = 4
EXPERTS_PER_GROUP = N_EXPERTS // N_EXPERT_GROUPS
EXPERT_FF = (11 * D_MODEL) // 16
SHARED_FF = EXPERT_FF
ROUTED_SCALE = 2.5
MOE_BLOCK = 256
NORM_EPS = 1e-6
N_MOD = 6

kernel_name = "hybrid_gqa_pool_moe_encoder_block"


def rms_norm(x, g):
    xf = x.astype(jnp.float32)
    y = xf * lax.rsqrt(jnp.mean(xf * xf, axis=-1, keepdims=True) + NORM_EPS)
    return (y * g.astype(jnp.float32)).astype(x.dtype)


def axial_rope_tables(seq_len):
    rows = seq_len // GRID_W
    row = jnp.broadcast_to(jnp.arange(rows, dtype=jnp.float32)[:, None], (rows, GRID_W)).reshape(seq_len)
    col = jnp.broadcast_to(jnp.arange(GRID_W, dtype=jnp.float32)[None, :], (rows, GRID_W)).reshape(seq_len)
    inv_freq = ROPE_THETA ** (-jnp.arange(0, ROPE_AXIS_DIM, 2, dtype=jnp.float32) / ROPE_AXIS_DIM)
    ang_r = row[:, None] * inv_freq[None, :]
    ang_c = col[:, None] * inv_freq[None, :]
    return jnp.cos(ang_r), jnp.sin(ang_r), jnp.cos(ang_c), jnp.sin(ang_c)


def _rotate(xh, cos, sin):
    x1, x2 = jnp.split(xh, 2, axis=-1)
    cos = cos[None, :, None, :]
    sin = sin[None, :, None, :]
    return jnp.concatenate([x1 * cos - x2 * sin, x2 * cos + x1 * sin], axis=-1)


def apply_axial_rope(x, tables):
    cos_r, sin_r, cos_c, sin_c = tables
    xf = x.astype(jnp.float32)
    xr, xc = jnp.split(xf, 2, axis=-1)
    out = jnp.concatenate([_rotate(xr, cos_r, sin_r), _rotate(xc, cos_c, sin_c)], axis=-1)
    return out.astype(x.dtype)


def gqa_attention(q, k, v):
    b, s = q.shape[0], q.shape[1]
    grp = N_Q_HEADS // N_KV_HEADS
    nqb = s // Q_BLOCK
    qb = q.reshape(b, nqb, Q_BLOCK, N_KV_HEADS, grp, HEAD_DIM).transpose(1, 0, 2, 3, 4, 5)
    scale = HEAD_DIM ** -0.5

    def one_block(qblk):
        sc = jnp.einsum('bqkgd,bskd->bkgqs', qblk, k, preferred_element_type=jnp.float32) * scale
        p = jax.nn.softmax(sc, axis=-1).astype(v.dtype)
        return jnp.einsum('bkgqs,bskd->bqkgd', p, v)

    o = lax.map(one_block, qb)
    return o.transpose(1, 0, 2, 3, 4, 5).reshape(b, s, Q_W)


def multiscale_pool(p, w_pool, pool_scale):
    b, s, _ = p.shape
    pg = p.reshape(b, s, N_POOL_GROUPS, POOL_GROUP_W).astype(jnp.float32)
    cs = jnp.concatenate([jnp.zeros((b, 1, N_POOL_GROUPS, POOL_GROUP_W), jnp.float32),
                          jnp.cumsum(pg, axis=1)], axis=1)
    t = jnp.arange(s)
    outs = []
    for gi, w in enumerate(POOL_WINDOWS):
        lo = jnp.clip(t - w // 2, 0, s)
        hi = jnp.clip(t - w // 2 + w, 0, s)
        csg = cs[:, :, gi]
        mean = (jnp.take(csg, hi, axis=1) - jnp.take(csg, lo, axis=1)) / (hi - lo).astype(jnp.float32)[None, :, None]
        outs.append(mean - pg[:, :, gi])
    pooled = jnp.stack(outs, axis=2).astype(p.dtype)
    mixed = jnp.einsum('bsgc,gcd->bsgd', pooled, w_pool).reshape(b, s, POOL_WIDTH)
    return mixed * pool_scale


def token_mixer(h, w_in, q_norm_g, k_norm_g, w_pool, pool_scale, w_out, rope_tables):
    b, s, _ = h.shape
    proj = h @ w_in
    q, k, v, p, ga, gp = jnp.split(proj, IN_SPLITS, axis=-1)
    q = apply_axial_rope(rms_norm(q.reshape(b, s, N_Q_HEADS, HEAD_DIM), q_norm_g), rope_tables)
    k = apply_axial_rope(rms_norm(k.reshape(b, s, N_KV_HEADS, HEAD_DIM), k_norm_g), rope_tables)
    v = v.reshape(b, s, N_KV_HEADS, HEAD_DIM)
    attn = gqa_attention(q, k, v)
    pool = multiscale_pool(p, w_pool, pool_scale)
    merged = jax.nn.sigmoid(ga) * attn + jax.nn.sigmoid(gp) * pool
    return merged @ w_out


def moe_ffn(h, w_router, router_bias, w_gate, w_up, w_down, ws_gate, ws_up, ws_down):
    b, s, d = h.shape
    n = b * s
    xf = h.reshape(n, d)
    scores = jax.nn.sigmoid(xf.astype(jnp.float32) @ w_router.astype(jnp.float32))
    choice = scores + router_bias.astype(jnp.float32)
    grp_scores = lax.top_k(choice.reshape(n, N_EXPERT_GROUPS, EXPERTS_PER_GROUP), 2)[0].sum(-1)
    gidx = lax.top_k(grp_scores, TOPK_GROUPS)[1]
    gmask = jax.nn.one_hot(gidx, N_EXPERT_GROUPS, dtype=jnp.float32).sum(1) > 0
    emask = jnp.repeat(gmask, EXPERTS_PER_GROUP, axis=1)
    idx = lax.top_k(jnp.where(emask, choice, -jnp.inf), TOP_K)[1]
    sel = jnp.take_along_axis(scores, idx, axis=1)
    wts = sel / jnp.sum(sel, axis=-1, keepdims=True) * ROUTED_SCALE

    a = n * TOP_K
    flat_e = idx.reshape(a).astype(jnp.int32)
    flat_tok = jnp.repeat(jnp.arange(n, dtype=jnp.int32), TOP_K)
    flat_w = wts.reshape(a)
    order = jnp.argsort(flat_e)
    se, stok, sw = flat_e[order], flat_tok[order], flat_w[order]
    counts = jnp.bincount(se, length=N_EXPERTS).astype(jnp.int32)
    start = jnp.cumsum(counts) - counts
    padded = ((counts + MOE_BLOCK - 1) // MOE_BLOCK) * MOE_BLOCK
    pend = jnp.cumsum(padded)
    pstart = pend - padded
    dest = pstart[se] + (jnp.arange(a, dtype=jnp.int32) - start[se])
    n_rows = ((a + N_EXPERTS * MOE_BLOCK + MOE_BLOCK - 1) // MOE_BLOCK) * MOE_BLOCK
    n_blocks = n_rows // MOE_BLOCK
    row_tok = jnp.zeros((n_rows,), jnp.int32).at[dest].set(stok)
    row_w = jnp.zeros((n_rows,), jnp.float32).at[dest].set(sw)
    blk_e = jnp.clip(jnp.searchsorted(pend, jnp.arange(n_blocks, dtype=jnp.int32) * MOE_BLOCK, side='right'),
                     0, N_EXPERTS - 1).astype(jnp.int32)

    def expert_block(args):
        rows, e = args
        xb = xf[rows]
        return (jax.nn.silu(xb @ w_gate[e]) * (xb @ w_up[e])) @ w_down[e]

    yr = lax.map(expert_block, (row_tok.reshape(n_blocks, MOE_BLOCK), blk_e)).reshape(n_rows, d)
    routed = jax.ops.segment_sum(yr.astype(jnp.float32) * row_w[:, None], row_tok, num_segments=n)
    shared = (jax.nn.silu(xf @ ws_gate) * (xf @ ws_up)) @ ws_down
    return (routed.astype(h.dtype) + shared).reshape(b, s, d)


def setup_inputs(seed: int = 0) -> dict:
    key = jax.random.key(seed)
    ks = jax.random.split(key, 24)
    f32 = jnp.float32
    nrm = lambda k, shape, sc: jax.random.normal(k, shape, f32) * sc
    L, D = DEPTH, D_MODEL
    return {
        "x": nrm(ks[0], (BATCH, SEQ, D), 1.0),
        "c": nrm(ks[1], (BATCH, D), 1.0),
        "w_mod": nrm(ks[2], (L, D, N_MOD * D), 0.5 * D ** -0.5),
        "b_mod": nrm(ks[3], (L, N_MOD * D), 0.01),
        "g_pre_mix": 1.0 + nrm(ks[4], (L, D), 0.05),
        "g_post_mix": 1.0 + nrm(ks[5], (L, D), 0.05),
        "g_pre_ffn": 1.0 + nrm(ks[6], (L, D), 0.05),
        "g_post_ffn": 1.0 + nrm(ks[7], (L, D), 0.05),
        "w_in": nrm(ks[8], (L, D, IN_W), D ** -0.5),
        "q_norm_g": 1.0 + nrm(ks[9], (L, HEAD_DIM), 0.05),
        "k_norm_g": 1.0 + nrm(ks[10], (L, HEAD_DIM), 0.05),
        "w_pool": nrm(ks[11], (L, N_POOL_GROUPS, POOL_GROUP_W, POOL_GROUP_W), POOL_GROUP_W ** -0.5),
        "pool_scale": 1.0 + nrm(ks[12], (L, POOL_WIDTH), 0.1),
        "w_out": nrm(ks[13], (L, D, D), D ** -0.5),
        "w_router": nrm(ks[14], (L, D, N_EXPERTS), D ** -0.5),
        "router_bias": nrm(ks[15], (L, N_EXPERTS), 0.01),
        "w_exp_gate": nrm(ks[16], (L, N_EXPERTS, D, EXPERT_FF), D ** -0.5),
        "w_exp_up": nrm(ks[17], (L, N_EXPERTS, D, EXPERT_FF), D ** -0.5),
        "w_exp_down": nrm(ks[18], (L, N_EXPERTS, EXPERT_FF, D), EXPERT_FF ** -0.5),
        "w_sh_gate": nrm(ks[19], (L, D, SHARED_FF), D ** -0.5),
        "w_sh_up": nrm(ks[20], (L, D, SHARED_FF), D ** -0.5),
        "w_sh_down": nrm(ks[21], (L, SHARED_FF, D), SHARED_FF ** -0.5),
    }


def reference(x, c, w_mod, b_mod, g_pre_mix, g_post_mix, g_pre_ffn, g_post_ffn, w_in, q_norm_g, k_norm_g,
              w_pool, pool_scale, w_out, w_router, router_bias, w_exp_gate, w_exp_up, w_exp_down,
              w_sh_gate, w_sh_up, w_sh_down):
    rope_tables = axial_rope_tables(x.shape[1])
    for l in range(DEPTH):
        mod = (jax.nn.silu(c.astype(jnp.float32)) @ w_mod[l].astype(jnp.float32)
               + b_mod[l].astype(jnp.float32)).astype(x.dtype)
        sh_a, sc_a, gt_a, sh_f, sc_f, gt_f = jnp.split(mod[:, None, :], N_MOD, axis=-1)
        h = rms_norm(x, g_pre_mix[l]) * (1 + sc_a) + sh_a
        y = token_mixer(h, w_in[l], q_norm_g[l], k_norm_g[l], w_pool[l], pool_scale[l], w_out[l], rope_tables)
        x = x + gt_a * rms_norm(y, g_post_mix[l])
        h = rms_norm(x, g_pre_ffn[l]) * (1 + sc_f) + sh_f
        y = moe_ffn(h, w_router[l], router_bias[l], w_exp_gate[l], w_exp_up[l], w_exp_down[l],
                    w_sh_gate[l], w_sh_up[l], w_sh_down[l])
        x = x + gt_f * rms_norm(y, g_post_ffn[l])
    return x
```

```python
import numpy as np
from contextlib import ExitStack
import concourse.bass as bass
import concourse.mybir as mybir
from concourse.bass_utils import run_bass_kernel_spmd

F32 = mybir.dt.float32
BF16 = mybir.dt.bfloat16
AF = mybir.ActivationFunctionType
ALU = mybir.AluOpType
AX = mybir.AxisListType

D = 2048
S_LEN = 8192
NOWN = 2048
NEXT = NOWN + 16
FF = 1408
NF = 11
NE = 64
EPS = 1e-6
ENG_NAMES = ("pe", "act", "dve", "pool", "sp")

DEBUG = {}


class Buf:
    __slots__ = ("name", "w", "r")

    def __init__(self, name):
        self.name = name
        self.w = None
        self.r = []


class Sched:
    def __init__(self, nc, ec):
        self.nc = nc
        self.ec = ec
        self.streams = {e: [] for e in ENG_NAMES}
        self.cnt = {}
        self.sems = {}
        self.waited = {e: {} for e in ENG_NAMES}
        for e in ("pe", "act", "dve", "pool"):
            self.new_sem(e)

    def new_sem(self, key):
        self.sems[key] = self.ec(self.nc.semaphore(key))
        self.cnt[key] = 0
        return key

    def _waits(self, eng, deps):
        need = {}
        for tok in deps:
            if tok is None:
                continue
            k, v = tok
            if v > need.get(k, 0):
                need[k] = v
        out = []
        wd = self.waited[eng]
        for k, v in need.items():
            if wd.get(k, 0) >= v:
                continue
            wd[k] = v
            out.append((k, v))
        return out

    @staticmethod
    def _deps(reads, writes):
        deps = []
        for b in reads:
            if b.w is not None:
                deps.append(b.w)
        for b in writes:
            if b.w is not None:
                deps.append(b.w)
            deps.extend(b.r)
        return deps

    @staticmethod
    def _commit(tok, reads, writes):
        for b in reads:
            b.r.append(tok)
            if len(b.r) > 64:
                best = {}
                for k, v in b.r:
                    if v > best.get(k, 0):
                        best[k] = v
                b.r = list(best.items())
        for b in writes:
            b.w = tok
            b.r = []

    def op(self, eng, emit, reads=(), writes=()):
        waits = self._waits(eng, self._deps(reads, writes))
        self.cnt[eng] += 1
        tok = (eng, self.cnt[eng])
        self.streams[eng].append((waits, emit, (eng, 1)))
        self._commit(tok, reads, writes)
        return tok

    def dma(self, queue, semkey, emit, reads=(), writes=()):
        waits = self._waits(queue, self._deps(reads, writes))
        self.cnt[semkey] += 16
        tok = (semkey, self.cnt[semkey])
        self.streams[queue].append((waits, emit, (semkey, 16)))
        self._commit(tok, reads, writes)
        return tok

    def barrier(self):
        allv = [(k, v) for k, v in self.cnt.items() if v > 0]
        for e in ENG_NAMES:
            waits = self._waits(e, allv)
            if waits:
                self.streams[e].append((waits, None, None))

    def replay(self, eng, eobj):
        sems = self.sems
        for waits, emit, inc in self.streams[eng]:
            for k, v in waits:
                eobj.wait_ge(sems[k], v)
            if emit is not None:
                ins = emit(eobj)
                ins.then_inc(sems[inc[0]], inc[1])


def build_program(stop_after=99, dbg=False):
    nc = bass.Bass("TRN2", target_bir_lowering=False)
    es = ExitStack()
    ec = es.enter_context

    def din(name, shape, dt=F32):
        return nc.dram_tensor(name, list(shape), dt, kind="ExternalInput").ap()

    xb = din("xb", [S_LEN, D])
    xe = din("xe", [NEXT, D])
    cT_d = din("cT", [128, 16])
    wmod_d = din("wmod_l", [24, 128, 16, 512])
    bmodT_d = din("bmodT", [128, 96])
    bgt_d = din("bgt_rep", [128, 2, D])
    gpost_d = din("gpost_rep", [128, 2, D])
    gpre_d = din("gpreT", [128, 2, 16])
    qkg_d = din("qkg", [128, 2])
    wk_d = din("wk_l", [128, 16, 512])
    wv_d = din("wv_l", [128, 16, 512])
    wq_d = din("wq_l", [16, 128, 16, 128])
    wp_d = din("wp_l", [16, 128, 16, 128])
    wga_d = din("wga_l", [16, 128, 16, 128])
    wgp_d = din("wgp_l", [16, 128, 16, 128])
    wpool_d = din("wpool_l", [4, 128, 4, 512])
    pscT_d = din("pscT", [128, 16])
    wout_d = din("wout_l", [128, 16, D])
    wr_d = din("wr_l", [128, 16, NE])
    rbias_d = din("rbias_rep", [128, NE])
    if stop_after >= 5:
        weg_d = din("weg_l", [NE + 1, NF, 128, 16, 128])
        weu_d = din("weu_l", [NE + 1, NF, 128, 16, 128])
        wed_d = din("wed_l", [NE + 1, 8, 128, NF, 256])
    ident_d = din("ident", [128, 128])
    perm_d = din("perm", [128, 128])
    cosk_d = din("cosk", [128, S_LEN])
    sink_d = din("sink", [128, S_LEN])
    cosq_d = din("cosq", [128, NOWN])
    sinq_d = din("sinq", [128, NOWN])
    hflag_d = din("hflag", [128, 2])
    iedge_d = din("iedge", [128, 4, 16])
    out_d = nc.dram_tensor("out", [NOWN, D], F32, kind="ExternalOutput").ap()
    kT_s = nc.dram_tensor("kT_s", [4, 128, S_LEN], BF16).ap()
    v_s = nc.dram_tensor("v_s", [4, 128, 64 * 128], BF16).ap()
    mrg_s = nc.dram_tensor("mrg_s", [16, 128, NOWN], BF16).ap()
    x1_s = nc.dram_tensor("x1_s", [NOWN, D], F32).ap()
    dbg_out = {}
    if dbg:
        dbg_out["d_hT"] = nc.dram_tensor("d_hT", [128, 16, NEXT], BF16, kind="ExternalOutput").ap()
        dbg_out["d_G"] = nc.dram_tensor("d_G", [128, 16, NE], F32, kind="ExternalOutput").ap()
        dbg_out["d_hT2"] = nc.dram_tensor("d_hT2", [128, 16, NOWN], BF16, kind="ExternalOutput").ap()
        dbg_out["d_kT"] = nc.dram_tensor("d_kT", [4, 128, S_LEN], BF16, kind="ExternalOutput").ap()
        dbg_out["d_v"] = nc.dram_tensor("d_v", [4, 128, 64 * 128], BF16, kind="ExternalOutput").ap()
        dbg_out["d_mrg"] = nc.dram_tensor("d_mrg", [16, 128, NOWN], BF16, kind="ExternalOutput").ap()
        dbg_out["d_x1"] = nc.dram_tensor("d_x1", [NOWN, D], F32, kind="ExternalOutput").ap()

    S = Sched(nc, ec)
    block = None

    sbc = [0]

    def sb(name, shape, dt, stack=None):
        sbc[0] += 1
        return (stack or es).enter_context(nc.sbuf_tensor(f"{name}_{sbc[0]}", list(shape), dt))

    pb = [ec(nc.psum_tensor(f"pb{i}", [128, 512], F32)) for i in range(8)]
    BP = [Buf(f"pb{i}") for i in range(8)]

    ident = sb("ident", [128, 128], F32)
    identb = sb("identb", [128, 128], BF16)
    permb = sb("permb", [128, 128], BF16)
    onesb = sb("onesb", [128, 128], BF16)
    ones32 = sb("ones32", [128, 128], F32)
    epsc = sb("epsc", [128, 1], F32)
    cT = sb("cT", [128, 16], F32)
    modT = sb("modT", [128, 96], F32)
    gpre = sb("gpre", [128, 2, 16], F32)
    A_a = sb("A_a", [128, 16], F32)
    A_f = sb("A_f", [128, 16], F32)
    qkg = sb("qkg", [128, 2], F32)
    pscT = sb("pscT", [128, 16], F32)
    hflag = sb("hflag", [128, 2], F32)
    iedge = sb("iedge", [128, 4, 16], F32)
    gg = sb("gg", [128, 2, D], F32)
    rbias = sb("rbias", [128, NE], F32)
    Bconst = Buf("const")
    Bmod = Buf("mod")
    Bgg = Buf("gg")

    ldc = S.new_sem("ldc")
    st_sem = S.new_sem("st")

    def dma_in(sem, out_ap, in_ap, writes, reads=(), queue="sp"):
        return S.dma(queue, sem, lambda e: e.dma_start(out=out_ap, in_=in_ap), reads=reads, writes=writes)

    def mm_group(out_ap, pairs, reads, writes):
        def emit(e):
            n = len(pairs)
            ins = None
            for i, (l, r) in enumerate(pairs):
                ins = e.matmul(out_ap, lhsT=l, rhs=r, start=(i == 0), stop=(i == n - 1))
            return ins
        return S.op("pe", emit, reads, writes)

    def act(out, in_, func, reads, writes, **kw):
        return S.op("act", lambda e: e.activation(out=out, in_=in_, func=func, **kw), reads, writes)

    def ts(eng, out, in0, s1, s2, op0, op1, reads, writes):
        if op1 is None:
            return S.op(eng, lambda e: e.tensor_scalar(out=out, in0=in0, scalar1=s1, scalar2=None, op0=op0), reads, writes)
        return S.op(eng, lambda e: e.tensor_scalar(out=out, in0=in0, scalar1=s1, scalar2=s2, op0=op0, op1=op1), reads, writes)

    def tt(eng, out, in0, in1, op, reads, writes):
        return S.op(eng, lambda e: e.tensor_tensor(out=out, in0=in0, in1=in1, op=op), reads, writes)

    def stt(eng, out, in0, scalar, in1, op0, op1, reads, writes):
        return S.op(eng, lambda e: e.scalar_tensor_tensor(out=out, in0=in0, scalar=scalar, in1=in1, op0=op0, op1=op1),
                    reads, writes)

    def cp(eng, out, in_, reads, writes):
        return S.op(eng, lambda e: e.tensor_copy(out=out, in_=in_), reads, writes)

    def rstd_from_ssq(ssq_ap, out_ap, Bssq, Bout, inv_n):
        act(out_ap, ssq_ap, AF.Sqrt, [Bssq, Bconst], [Bout], bias=epsc[:, 0:1], scale=inv_n)
        S.op("dve", lambda e: e.reciprocal(out=out_ap, in_=out_ap), [Bout], [Bout])

    ph0 = ExitStack()
    for (t, d_) in ((ident, ident_d), (cT, cT_d), (modT, bmodT_d), (gpre, gpre_d), (qkg, qkg_d), (pscT, pscT_d),
                    (hflag, hflag_d), (iedge, iedge_d), (rbias, rbias_d)):
        dma_in(ldc, t[:], d_, [Bconst])
    perm32 = sb("perm32", [128, 128], F32, ph0)
    dma_in(ldc, perm32[:], perm_d, [Bconst])
    bgt = sb("bgt", [128, 2, D], F32, ph0)
    gpo = sb("gpo", [128, 2, D], F32, ph0)
    dma_in(ldc, bgt[:], bgt_d, [Bconst])
    dma_in(ldc, gpo[:], gpost_d, [Bconst])
    Bconst.w = (ldc, S.cnt[ldc])
    S.op("dve", lambda e: e.memset(onesb[:], 1.0), [], [Bconst])
    S.op("dve", lambda e: e.memset(ones32[:], 1.0), [], [Bconst])
    S.op("dve", lambda e: e.memset(epsc[:], EPS), [], [Bconst])
    cp("dve", identb[:], ident[:], [Bconst], [Bconst])
    cp("dve", permb[:], perm32[:], [Bconst], [Bconst])
    act(cT[:], cT[:], AF.Silu, [Bconst], [Bconst])
    cTb = sb("cTb", [128, 16, 128], F32, ph0)
    for kc in range(16):
        act(cTb[:, kc, :], ones32[:], AF.Copy, [Bconst], [Bconst], scale=cT[:, kc:kc + 1])
    wm = [sb(f"wm{i}", [128, 16, 512], F32, ph0) for i in range(2)]
    Bwm = [Buf(f"wm{i}") for i in range(2)]
    wm_sem = [S.new_sem(f"wm{i}") for i in range(2)]
    Bpm = BP[0]
    first_fm = True
    fm_blocks = list(range(0, 8)) + list(range(12, 20))
    for blk in range(24):
        s = blk % 2
        dma_in(wm_sem[s], wm[s][:], wmod_d[blk], [Bwm[s]])
        if blk in fm_blocks:
            for jj in range(4):
                j = blk * 4 + jj
                pairs = [(wm[s][:, kc, jj * 128:(jj + 1) * 128], cT[:, kc:kc + 1]) for kc in range(16)]
                mm_group(pb[0][:, j:j + 1], pairs, [Bwm[s], Bconst], [Bpm])
        else:
            gi = 0 if blk < 12 else 1
            cb = (blk - 8) if blk < 12 else (blk - 20)
            pairs = [(cTb[:, kc, :], wm[s][:, kc, :]) for kc in range(16)]
            bank = 1 + (blk % 2)
            mm_group(pb[bank][:], pairs, [Bwm[s], Bconst], [BP[bank]])
            sl = slice(cb * 512, (cb + 1) * 512)
            tt("dve", gg[:, gi, sl], pb[bank][:], bgt[:, gi, sl], ALU.add, [BP[bank], Bconst], [Bgg])
            tt("dve", gg[:, gi, sl], gg[:, gi, sl], gpo[:, gi, sl], ALU.mult, [Bgg, Bconst], [Bgg])
    for (a, b_) in ((0, 32), (48, 80)):
        tt("dve", modT[:, a:b_], pb[0][:, a:b_], modT[:, a:b_], ALU.add, [Bpm, Bconst], [Bmod])
    stt("dve", A_a[:], modT[:, 16:32], 1.0, gpre[:, 0, :], ALU.add, ALU.mult, [Bmod, Bconst], [Bmod])
    stt("dve", A_f[:], modT[:, 64:80], 1.0, gpre[:, 1, :], ALU.add, ALU.mult, [Bmod, Bconst], [Bmod])
    sh_a = modT[:, 0:16]
    sh_f = modT[:, 48:64]
    S.barrier()
    ph0.close()

    def norm_transpose_group(stack_bufs, x_src_ap, dst_fn, Bdst, Aap, shap, tbanks):
        (xt, Bxt, xsem, ssq, Bssq, xn, Bxn, junk, Bjunk) = stack_bufs
        dma_in(xsem, xt[:], x_src_ap, [Bxt])
        for t in range(4):
            act(junk[:], xt[:, t, :], AF.Square, [Bxt], [Bjunk, Bssq], accum_out=ssq[:, t:t + 1])
        rstd_from_ssq(ssq[:, 0:4], ssq[:, 4:8], Bssq, Bssq, 1.0 / D)
        for t in range(4):
            if t % 2 == 0:
                ts("dve", xn[:, t, :], xt[:, t, :], ssq[:, 4 + t:5 + t], None, ALU.mult, None, [Bxt, Bssq], [Bxn])
            else:
                act(xn[:, t, :], xt[:, t, :], AF.Copy, [Bxt, Bssq], [Bxn], scale=ssq[:, 4 + t:5 + t])
        for c in range(16):
            bk = tbanks[c % 2]

            def emit(e, c=c, bk=bk):
                ins = None
                for t in range(4):
                    ins = e.transpose(pb[bk][:, t * 128:(t + 1) * 128], xn[:, t, c * 128:(c + 1) * 128], ident[:])
                return ins
            S.op("pe", emit, [Bxn, Bconst], [BP[bk]])
            act(dst_fn(c), pb[bk][:], AF.Identity, [BP[bk], Bmod], [Bdst], bias=shap[:, c:c + 1], scale=Aap[:, c:c + 1])

    def qk_post(psrc_bank, gcol, cos_ap, sin_ap, Btab, out_ap, Bout, tmp, Btmp, abank, rbank):
        (sq, tg, u, w, rs) = tmp
        ps = pb[psrc_bank]
        lim = DEBUG.get("qkstage", 99)
        if lim <= 0:
            return
        act(sq[:], ps[:], AF.Copy, [BP[psrc_bank]], [Btmp[0]])
        stt("dve", sq[:], ps[:], 1.0, sq[:], ALU.mult, ALU.mult, [BP[psrc_bank], Btmp[0]], [Btmp[0]])
        if lim <= 1:
            return
        ts("dve", tg[:], ps[:], qkg[:, gcol:gcol + 1], None, ALU.mult, None, [BP[psrc_bank], Bconst], [Btmp[1]])
        if lim <= 2:
            return
        mm_group(pb[abank][:], [(onesb[:], sq[:])], [Btmp[0], Bconst], [BP[abank]])
        if lim <= 3:
            return
        mm_group(pb[rbank][:], [(permb[:], tg[:])], [Btmp[1], Bconst], [BP[rbank]])
        if lim <= 4:
            return
        act(rs[:], pb[abank][:], AF.Sqrt, [BP[abank], Bconst], [Btmp[4]], bias=epsc[:, 0:1], scale=1.0 / 128)
        if lim <= 5:
            return
        S.op("dve", lambda e: e.reciprocal(out=rs[:], in_=rs[:]), [Btmp[4]], [Btmp[4]])
        if lim <= 6:
            return
        tt(DEBUG.get("qkeng", "pool"), u[:], tg[:], cos_ap, ALU.mult, [Btmp[1], Btab], [Btmp[2]])
        if lim <= 7:
            return
        tt("dve", w[:], pb[rbank][:], sin_ap, ALU.mult, [BP[rbank], Btab], [Btmp[3]])
        if lim <= 8:
            return
        tt(DEBUG.get("qkeng", "pool"), u[:], u[:], w[:], ALU.add, [Btmp[2], Btmp[3]], [Btmp[2]])
        if lim <= 9:
            return
        tt("dve", out_ap, u[:], rs[:], ALU.mult, [Btmp[2], Btmp[4]], [Bout])

    ph1 = ExitStack()
    xt1 = sb("xt", [128, 4, D], F32, ph1)
    xt = [xt1, xt1]
    Bxt1 = Buf("xt")
    Bxt = [Bxt1, Bxt1]
    xs1 = S.new_sem("xs")
    xsem = [xs1, xs1]
    ssq = [sb(f"ssq{i}", [128, 8], F32, ph1) for i in range(2)]
    Bssq = [Buf(f"ssq{i}") for i in range(2)]
    xn = sb("xn", [128, 4, D], F32, ph1)
    Bxn = Buf("xn")
    junk = sb("junk", [128, D], BF16, ph1)
    Bjunk = Buf("junk")
    hTg = sb("hTg", [128, 16, 512], BF16, ph1)
    BhTg = Buf("hTg")
    wk = sb("wk", [128, 16, 512], BF16, ph1)
    wv = sb("wv", [128, 16, 512], BF16, ph1)
    Bwkv = Buf("wkv")
    wsem = S.new_sem("wkv")
    dma_in(wsem, wk[:], wk_d, [Bwkv], queue="pool")
    dma_in(wsem, wv[:], wv_d, [Bwkv], queue="pool")
    ctab = [sb(f"ctab{i}", [128, 2, 512], F32, ph1) for i in range(2)]
    Bctab = [Buf(f"ctab{i}") for i in range(2)]
    csem = [S.new_sem(f"cs{i}") for i in range(2)]
    qtmp = (sb("q_sq", [128, 512], BF16, ph1), sb("q_tg", [128, 512], BF16, ph1), sb("q_u", [128, 512], F32, ph1),
            sb("q_w", [128, 512], F32, ph1), sb("q_rs", [128, 512], F32, ph1))
    Bqtmp = [Buf(f"qtmp{i}") for i in range(5)]
    kTg = [sb(f"kTg{i}", [128, 4, 512], BF16, ph1) for i in range(2)]
    BkTg = [Buf(f"kTg{i}") for i in range(2)]
    vg = [sb(f"vg{i}", [128, 4, 4, 128], BF16, ph1) for i in range(2)]
    Bvg = [Buf(f"vg{i}") for i in range(2)]
    kv_st = S.new_sem("kvst")
    Bks = Buf("kT_s")
    Bvs = Buf("v_s")

    for g in range(DEBUG.get('kv_groups', 16) if stop_after >= 1 else 0):
        s = g % 2
        dma_in(csem[s], ctab[s][:, 0, :], cosk_d[:, g * 512:(g + 1) * 512], [Bctab[s]])
        dma_in(csem[s], ctab[s][:, 1, :], sink_d[:, g * 512:(g + 1) * 512], [Bctab[s]])
        norm_transpose_group((xt[s], Bxt[s], xsem[s], ssq[s], Bssq[s], xn, Bxn, junk, Bjunk),
                             xb[g * 512:(g + 1) * 512, :].rearrange("(t p) d -> p t d", p=128),
                             lambda c: hTg[:, c, :], BhTg, A_a, sh_a, (0, 1))
        for hd in range(0 if DEBUG.get("nok") else 4):
            bk = 2 + (hd % 2)
            mm_group(pb[bk][:], [(wk[:, kc, hd * 128:(hd + 1) * 128], hTg[:, kc, :]) for kc in range(16)],
                     [Bwkv, BhTg], [BP[bk]])
            qk_post(bk, 1, ctab[s][:, 0, :], ctab[s][:, 1, :], Bctab[s], kTg[s][:, hd, :], BkTg[s], qtmp, Bqtmp, 6, 7)
        for t in range(0 if DEBUG.get("nov") else 4):
            bk = 4 + (t % 2)
            mm_group(pb[bk][:], [(hTg[:, kc, t * 128:(t + 1) * 128], wv[:, kc, :]) for kc in range(16)],
                     [Bwkv, BhTg], [BP[bk]])
            act(vg[s][:, :, t, :], pb[bk][:].rearrange("p (h d) -> p h d", h=4), AF.Copy, [BP[bk]], [Bvg[s]])
        for hd in range(0 if DEBUG.get("nostore") else 4):
            dma_in(kv_st, kT_s[hd, :, g * 512:(g + 1) * 512], kTg[s][:, hd, :], [Bks], reads=[BkTg[s]])
            dma_in(kv_st, v_s[hd, :, g * 512:(g + 1) * 512], vg[s][:, hd, :, :].rearrange("p t d -> p (t d)"),
                   [Bvs], reads=[Bvg[s]])

    if dbg:
        dma_in(st_sem, dbg_out["d_kT"], kT_s, [], reads=[Bks])
        dma_in(st_sem, dbg_out["d_v"], v_s, [], reads=[Bvs])
    S.barrier()
    ph1.close()
    ph23 = ExitStack()
    hT = sb("hT", [128, 16, NEXT], BF16, ph23)
    BhT = Buf("hT")
    ph1 = ExitStack()
    xt1 = sb("xt", [128, 4, D], F32, ph1)
    xt = [xt1, xt1]
    Bxt1 = Buf("xt")
    Bxt = [Bxt1, Bxt1]
    ssq = [sb(f"ssq{i}", [128, 8], F32, ph1) for i in range(2)]
    Bssq = [Buf(f"ssq{i}") for i in range(2)]
    xn = sb("xn", [128, 4, D], F32, ph1)
    Bxn = Buf("xn")
    junk = sb("junk", [128, D], BF16, ph1)
    Bjunk = Buf("junk")
    for g in range(4 if stop_after >= 2 else 0):
        s = g % 2
        norm_transpose_group((xt[s], Bxt[s], xsem[s], ssq[s], Bssq[s], xn, Bxn, junk, Bjunk),
                             xe[8 + g * 512:8 + (g + 1) * 512, :].rearrange("(t p) d -> p t d", p=128),
                             lambda c, g=g: hT[:, c, 8 + g * 512:8 + (g + 1) * 512], BhT, A_a, sh_a, (0, 1))
    xh = sb("xh", [16, D], F32, ph1)
    Bxh = Buf("xh")
    hsem = S.new_sem("halo")
    dma_in(hsem, xh[0:8, :], xe[0:8, :], [Bxh])
    dma_in(hsem, xh[8:16, :], xe[8 + NOWN:16 + NOWN, :], [Bxh])
    sqh = sb("sqh", [16, 2], F32, ph1)
    Bsqh = Buf("sqh")
    xnh = sb("xnh", [16, D], F32, ph1)
    act(junk[0:16, :], xh[:], AF.Square, [Bxh], [Bjunk, Bsqh], accum_out=sqh[:, 0:1])
    act(sqh[:, 1:2], sqh[:, 0:1], AF.Sqrt, [Bsqh, Bconst], [Bsqh], bias=epsc[0:16, 0:1], scale=1.0 / D)
    S.op("dve", lambda e: e.reciprocal(out=sqh[:, 1:2], in_=sqh[:, 1:2]), [Bsqh], [Bsqh])
    ts("dve", xnh[:], xh[:], sqh[:, 1:2], None, ALU.mult, None, [Bxh, Bsqh], [Bxh])

    def emit_h(e):
        ins = None
        for c in range(16):
            ins = e.transpose(pb[0][:, c * 16:(c + 1) * 16], xnh[:, c * 128:(c + 1) * 128], ident[0:16, 0:16])
        return ins
    S.op("pe", emit_h, [Bxh, Bconst], [BP[0]])
    htmp = sb("htmp", [128, 16, 16], F32, ph1)
    Bhtmp = Buf("htmp")
    for c in range(16):
        act(htmp[:, c, :], pb[0][:, c * 16:(c + 1) * 16], AF.Identity, [BP[0], Bmod], [Bhtmp],
            bias=sh_a[:, c:c + 1], scale=A_a[:, c:c + 1])
    ts("dve", hT[:, :, 0:8], htmp[:, :, 0:8], hflag[:, 0:1], None, ALU.mult, None, [Bhtmp, Bconst], [BhT])
    ts("dve", hT[:, :, 8 + NOWN:16 + NOWN], htmp[:, :, 8:16], hflag[:, 1:2], None, ALU.mult, None, [Bhtmp, Bconst], [BhT])
    if dbg:
        dma_in(st_sem, dbg_out["d_hT"], hT[:], [], reads=[BhT])
    S.barrier()
    ph1.close()

    if stop_after >= 3:
        ph3 = ExitStack()
        kTh = sb("kTh", [128, S_LEN], BF16, ph3)
        vh = sb("vh", [128, 64, 128], BF16, ph3)
        Bkv = Buf("kvh")
        kvsem = S.new_sem("kvld")
        cq = sb("cq", [128, 2, NOWN], F32, ph3)
        Bcq = Buf("cq")
        dma_in(ldc, cq[:, 0, :], cosq_d, [Bcq])
        dma_in(ldc, cq[:, 1, :], sinq_d, [Bcq])
        qT = sb("qT", [128, NOWN], BF16, ph3)
        BqT = Buf("qT")
        wsl = [sb(f"wsl{i}", [128, 16, 128], BF16, ph3) for i in range(3)]
        Bwsl = [Buf(f"wsl{i}") for i in range(3)]
        wslsem = [S.new_sem(f"wsl{i}") for i in range(3)]
        wslc = [0]

        def load_w(src):
            i = wslc[0] % 3
            wslc[0] += 1
            dma_in(wslsem[i], wsl[i][:], src, [Bwsl[i]], queue="pool")
            return wsl[i], Bwsl[i]
        pT = [sb(f"pT{i}", [128, 512], BF16, ph3) for i in range(3)]
        BpT = [Buf(f"pT{i}") for i in range(3)]
        wpl = sb("wpl", [128, 4, 512], BF16, ph3)
        Bwpl = Buf("wpl")
        wplsem = S.new_sem("wpl")
        pooled = sb("pooled", [128, 4, NOWN], BF16, ph3)
        Bpooled = Buf("pooled")
        mrgh = [sb(f"mrgh{i}", [128, NOWN], BF16, ph3) for i in range(2)]
        Bmrgh = [Buf(f"mrgh{i}") for i in range(2)]
        phB = None
        mst = S.new_sem("mst")
        Bmrgs = Buf("mrg_s")
        ATT_SCALE = 128.0 ** -0.5
        pti = 0
        for h in range(16):
            kvh, gi = h // 4, h // 4
            if h % 4 == 0:
                dma_in(kvsem, kTh[:], kT_s[kvh], [Bkv], reads=[Bks])
                dma_in(kvsem, vh[:].rearrange("p t d -> p (t d)"), v_s[kvh], [Bkv], reads=[Bvs])
                if phB is not None:
                    S.barrier()
                    phB.close()
                phA = ExitStack()
                pc = sb("pc", [128, NEXT], F32, phA)
                Bpc = Buf("pc")
                sA = sb("sA", [128, NEXT], F32, phA)
                sBt = sb("sBt", [128, NEXT], F32, phA)
                BsA = Buf("sA")
                BsB = Buf("sB")
                dma_in(wplsem, wpl[:], wpool_d[gi], [Bwpl], queue="pool")
                w = (2, 4, 8, 16)[gi]
                for cc in range(4):
                    wp_t, Bwp = load_w(wp_d[gi * 4 + cc])
                    col = 0
                    ci = 0
                    while col < NEXT:
                        n = min(512, NEXT - col)
                        bk = 6 + (ci % 2)
                        mm_group(pb[bk][:, 0:n], [(wp_t[:, kc, :], hT[:, kc, col:col + n]) for kc in range(16)],
                                 [Bwp, BhT], [BP[bk]])
                        act(pc[:, col:col + n], pb[bk][:, 0:n], AF.Copy, [BP[bk]], [Bpc])
                        col += n
                        ci += 1
                    tt("dve", sA[:, 1:NEXT], pc[:, 1:NEXT], pc[:, 0:NEXT - 1], ALU.add, [Bpc], [BsA])
                    cur, Bcur, oth, Both = sA, BsA, sBt, BsB
                    lo, hi = 1, NEXT
                    sh = 1
                    ww = 2
                    while ww < w:
                        nlo, nhi = lo + sh, hi - sh
                        tt("pool", oth[:, nlo:nhi], cur[:, nlo - sh:nhi - sh], cur[:, nlo + sh:nhi + sh], ALU.add,
                           [Bcur], [Both])
                        cur, Bcur, oth, Both = oth, Both, cur, Bcur
                        lo, hi = nlo, nhi
                        sh *= 2
                        ww *= 2
                    stt("dve", pooled[:, cc, :], cur[:, 8:8 + NOWN], 1.0 / w, pc[:, 8:8 + NOWN], ALU.mult, ALU.subtract,
                        [Bcur, Bpc], [Bpooled])
                    for (c0, e0) in ((0, 0), (NOWN - 8, 8)):
                        tt("dve", oth[:, 0:8], cur[:, 8 + c0:16 + c0], iedge[:, gi, e0:e0 + 8], ALU.mult,
                           [Bcur, Bconst], [Both])
                        tt("dve", pooled[:, cc, c0:c0 + 8], oth[:, 0:8], pc[:, 8 + c0:16 + c0], ALU.subtract,
                           [Both, Bpc], [Bpooled])
                S.barrier()
                phA.close()
                phB = ExitStack()
                qtmp = (sb("q_sq", [128, 512], BF16, phB), sb("q_tg", [128, 512], BF16, phB),
                        sb("q_u", [128, 512], F32, phB), sb("q_w", [128, 512], F32, phB), sb("q_rs", [128, 512], F32, phB))
                Bqtmp = [Buf(f"qtmp{i}") for i in range(5)]
                oT = sb("oT", [128, 512], F32, phB)
                BoT = Buf("oT")
                rden = sb("rden", [128, 512], F32, phB)
                Brden = Buf("rden")
                sga = sb("sga", [128, 512], F32, phB)
                sgp = sb("sgp", [128, 512], F32, phB)
                Bsga = Buf("sga")
                Bsgp = Buf("sgp")
            wq_t, Bwq = load_w(wq_d[h])
            wga_t, Bwga = load_w(wga_d[h])
            wgp_t, Bwgp = load_w(wgp_d[h])
            ms = h % 2
            for qg in range(4):
                bk = 6
                mm_group(pb[bk][:], [(wq_t[:, kc, :], hT[:, kc, 8 + qg * 512:8 + (qg + 1) * 512]) for kc in range(16)],
                         [Bwq, BhT], [BP[bk]])
                qk_post(bk, 0, cq[:, 0, qg * 512:(qg + 1) * 512], cq[:, 1, qg * 512:(qg + 1) * 512], Bcq,
                        qT[:, qg * 512:(qg + 1) * 512], BqT, qtmp, Bqtmp, 7, 6)
            for qg in range(4):
                ob, db = (2, 3) if qg % 2 == 0 else (4, 5)
                qs = qT[:, qg * 512:(qg + 1) * 512]
                for kt in range(64):
                    sbk = kt % 2
                    mm_group(pb[sbk][:], [(kTh[:, kt * 128:(kt + 1) * 128], qs)], [Bkv, BqT], [BP[sbk]])
                    p_i = pti % 3
                    pti += 1
                    act(pT[p_i][:], pb[sbk][:], AF.Exp, [BP[sbk]], [BpT[p_i]], scale=ATT_SCALE)

                    def emit(e, kt=kt, p_i=p_i, ob=ob, db=db):
                        e.matmul(pb[ob][:], lhsT=vh[:, kt, :], rhs=pT[p_i][:], start=(kt == 0), stop=(kt == 63))
                        return e.matmul(pb[db][:], lhsT=onesb[:], rhs=pT[p_i][:], start=(kt == 0), stop=(kt == 63))
                    S.op("pe", emit, [Bkv, BpT[p_i], Bconst], [BP[ob], BP[db]])
                S.op("dve", lambda e, db=db, rden=rden: e.reciprocal(out=rden[:], in_=pb[db][:]), [BP[db]], [Brden])
                tt("dve", oT[:], pb[ob][:], rden[:], ALU.mult, [BP[ob], Brden], [BoT])
                cols = slice(8 + qg * 512, 8 + (qg + 1) * 512)
                oc = slice(qg * 512, (qg + 1) * 512)
                mm_group(pb[6][:], [(wga_t[:, kc, :], hT[:, kc, cols]) for kc in range(16)], [Bwga, BhT], [BP[6]])
                act(sga[:], pb[6][:], AF.Sigmoid, [BP[6]], [Bsga])
                mm_group(pb[7][:], [(wgp_t[:, kc, :], hT[:, kc, cols]) for kc in range(16)], [Bwgp, BhT], [BP[7]])
                act(sgp[:], pb[7][:], AF.Sigmoid, [BP[7]], [Bsgp])
                cc = h % 4
                mm_group(pb[6][:], [(wpl[:, kc, cc * 128:(cc + 1) * 128], pooled[:, kc, oc]) for kc in range(4)],
                         [Bwpl, Bpooled], [BP[6]])
                stt("dve", sgp[:], pb[6][:], pscT[:, h:h + 1], sgp[:], ALU.mult, ALU.mult, [BP[6], Bconst, Bsgp], [Bsgp])
                tt("pool", sga[:], sga[:], oT[:], ALU.mult, [Bsga, BoT], [Bsga])
                tt("dve", mrgh[ms][:, oc], sga[:], sgp[:], ALU.add, [Bsga, Bsgp], [Bmrgh[ms]])
            dma_in(mst, mrg_s[h], mrgh[ms][:], [Bmrgs], reads=[Bmrgh[ms]])
        if dbg:
            dma_in(st_sem, dbg_out["d_mrg"], mrg_s, [], reads=[Bmrgs])
        S.barrier()
        phB.close()
        ph3.close()
    ph23.close()
    es_hT_done = True

    if stop_after >= 4:
        ph4 = ExitStack()
        hT2 = sb("hT2", [128, 16, 1024], BF16, ph4)
        BhT2 = Buf("hT2")
        G = sb("G", [128, 8, NE + 1], F32, ph4)
        BG = Buf("G")
        wrt = sb("wrt", [128, 16, NE], F32, ph4)
        dma_in(ldc, wrt[:], wr_d, [Bconst])
        Bconst.w = (ldc, S.cnt[ldc])
        Bx1s = Buf("x1_s")
        x1st = S.new_sem("x1st")
        for half in range(2):
            p4 = ExitStack()
            wout = sb("wout", [128, 16, D], BF16, p4)
            Bwout = Buf("wout")
            wosem = S.new_sem(f"wo{half}")
            for q4 in range(4):
                dma_in(wosem, wout[:, q4 * 4:(q4 + 1) * 4, :], wout_d[:, q4 * 4:(q4 + 1) * 4, :], [Bwout], queue="pool")
            mt = [sb(f"mt{i}", [128, 16, 128], BF16, p4) for i in range(2)]
            Bmt = [Buf(f"mt{i}") for i in range(2)]
            mtsem = [S.new_sem(f"mt{half}{i}") for i in range(2)]
            xo = [sb(f"xo{i}", [128, D], F32, p4) for i in range(2)]
            Bxo = [Buf(f"xo{i}") for i in range(2)]
            xosem = [S.new_sem(f"xo{half}{i}") for i in range(2)]
            tmp = sb("tmp4", [128, D], F32, p4)
            Btmp = Buf("tmp4")
            x1t = sb("x1t", [128, D], F32, p4)
            Bx1t = Buf("x1t")
            xn2 = sb("xn2", [128, D], F32, p4)
            Bxn2 = Buf("xn2")
            junk4 = sb("junk4", [128, D], BF16, p4)
            Bjunk4 = Buf("junk4")
            h2f = sb("h2f", [128, 16, 128], F32, p4)
            Bh2f = Buf("h2f")
            st4 = sb("st4", [128, 16], F32, p4)
            Bst4 = Buf("st4")
            rt = {n: sb(f"rt_{n}", shp, F32, p4) for n, shp in
                  (("sc", [128, NE]), ("ch", [128, NE]), ("m8", [128, 8, 8]), ("gs", [128, 8]), ("g8", [128, 8]),
                   ("gm", [128, 8]), ("mch", [128, NE]), ("t8", [128, 8]), ("sel", [128, NE]), ("den", [128, 2]))}
            Brt = Buf("rt")
            for tl in range(8):
                tg_ = half * 8 + tl
                s = tl % 2
                dma_in(mtsem[s], mt[s][:], mrg_s[:, :, tg_ * 128:(tg_ + 1) * 128].rearrange("c p t -> p c t"),
                       [Bmt[s]], reads=[Bmrgs] if stop_after >= 3 else [])
                dma_in(xosem[s], xo[s][:], xe[8 + tg_ * 128:8 + (tg_ + 1) * 128, :], [Bxo[s]])
                for cg in range(4):
                    mm_group(pb[cg][:], [(mt[s][:, kc, :], wout[:, kc, cg * 512:(cg + 1) * 512]) for kc in range(16)],
                             [Bmt[s], Bwout], [BP[cg]])
                for cg in range(4):
                    act(tmp[:, cg * 512:(cg + 1) * 512], pb[cg][:], AF.Copy, [BP[cg]], [Btmp])
                act(junk4[:], tmp[:], AF.Square, [Btmp], [Bjunk4, Bst4], accum_out=st4[:, 4:5])
                rstd_from_ssq(st4[:, 4:5], st4[:, 5:6], Bst4, Bst4, 1.0 / D)
                stt("dve", tmp[:], tmp[:], st4[:, 5:6], gg[:, 0, :], ALU.mult, ALU.mult, [Btmp, Bst4, Bgg], [Btmp])
                tt("pool", x1t[:], tmp[:], xo[s][:], ALU.add, [Btmp, Bxo[s]], [Bx1t])
                dma_in(x1st, x1_s[tg_ * 128:(tg_ + 1) * 128, :], x1t[:], [Bx1s], reads=[Bx1t])
                act(junk4[:], x1t[:], AF.Square, [Bx1t], [Bjunk4, Bst4], accum_out=st4[:, 6:7])
                rstd_from_ssq(st4[:, 6:7], st4[:, 7:8], Bst4, Bst4, 1.0 / D)
                ts("dve", xn2[:], x1t[:], st4[:, 7:8], None, ALU.mult, None, [Bx1t, Bst4], [Bxn2])
                for q4 in range(4):
                    bk = 4 + q4

                    def emit(e, q4=q4, bk=bk):
                        ins = None
                        for j in range(4):
                            c = q4 * 4 + j
                            ins = e.transpose(pb[bk][:, j * 128:(j + 1) * 128], xn2[:, c * 128:(c + 1) * 128], ident[:])
                        return ins
                    S.op("pe", emit, [Bxn2, Bconst], [BP[bk]])
                    for j in range(4):
                        c = q4 * 4 + j
                        act(h2f[:, c, :], pb[bk][:, j * 128:(j + 1) * 128], AF.Identity, [BP[bk], Bmod], [Bh2f],
                            bias=sh_f[:, c:c + 1], scale=A_f[:, c:c + 1])
                cp("pool", hT2[:, :, tl * 128:(tl + 1) * 128], h2f[:], [Bh2f], [BhT2])
                mm_group(pb[0][:, 0:NE], [(h2f[:, c, :], wrt[:, c, :]) for c in range(16)], [Bh2f, Bconst], [BP[0]])
                act(rt["sc"][:], pb[0][:, 0:NE], AF.Sigmoid, [BP[0]], [Brt])
                tt("dve", rt["ch"][:], rt["sc"][:], rbias[:], ALU.add, [Brt, Bconst], [Brt])
                for g8 in range(8):
                    S.op("dve", lambda e, g8=g8: e.max(out=rt["m8"][:, g8, :], in_=rt["ch"][:, g8 * 8:(g8 + 1) * 8]),
                         [Brt], [Brt])
                tt("dve", rt["gs"][:], rt["m8"][:, :, 0], rt["m8"][:, :, 1], ALU.add, [Brt], [Brt])
                S.op("dve", lambda e: e.max(out=rt["g8"][:], in_=rt["gs"][:]), [Brt], [Brt])
                ts("dve", rt["gm"][:], rt["gs"][:], rt["g8"][:, 3:4], None, ALU.is_ge, None, [Brt], [Brt])
                ts("dve", rt["gm"][:], rt["gm"][:], 10.0, -10.0, ALU.mult, ALU.add, [Brt], [Brt])
                tt("dve", rt["mch"][:].rearrange("p (g e) -> p g e", g=8), rt["ch"][:].rearrange("p (g e) -> p g e", g=8),
                   rt["gm"][:].unsqueeze(2).to_broadcast([128, 8, 8]), ALU.add, [Brt], [Brt])
                S.op("dve", lambda e: e.max(out=rt["t8"][:], in_=rt["mch"][:]), [Brt], [Brt])
                ts("dve", rt["sel"][:], rt["mch"][:], rt["t8"][:, 5:6], None, ALU.is_ge, None, [Brt], [Brt])
                tt("dve", rt["sel"][:], rt["sel"][:], rt["sc"][:], ALU.mult, [Brt], [Brt])
                S.op("dve", lambda e: e.reduce_sum(out=rt["den"][:, 0:1], in_=rt["sel"][:], axis=AX.X), [Brt], [Brt])
                S.op("dve", lambda e: e.reciprocal(out=rt["den"][:, 1:2], in_=rt["den"][:, 0:1]), [Brt], [Brt])
                ts("dve", G[:, tl, 0:NE], rt["sel"][:], rt["den"][:, 1:2], 2.5, ALU.mult, ALU.mult, [Brt], [BG])
                S.op("dve", lambda e, tl=tl: e.memset(G[:, tl, NE:NE + 1], 1.0), [], [BG])
            if dbg:
                dma_in(st_sem, dbg_out["d_hT2"][:, :, half * 1024:(half + 1) * 1024], hT2[:], [], reads=[BhT2])
                dma_in(st_sem, dbg_out["d_G"][:, half * 8:(half + 1) * 8, :], G[:, :, 0:NE], [], reads=[BG])
            S.barrier()
            p4.close()
            if stop_after < 5:
                continue
            p5 = ExitStack()
            acc = sb("acc", [128, 8, D], F32, p5)
            Bacc = [Buf(f"acc{i}") for i in range(8)]
            p5a = ExitStack()
            NGU = 4
            wgr = [sb(f"wgr{i}", [128, 16, 128], BF16, p5a) for i in range(NGU)]
            wur = [sb(f"wur{i}", [128, 16, 128], BF16, p5a) for i in range(NGU)]
            Bgu = [Buf(f"gu{i}") for i in range(NGU)]
            gusem = [S.new_sem(f"gu{half}{i}") for i in range(NGU)]
            NDW = 3
            wdr = [sb(f"wdr{i}", [128, NF, 256], BF16, p5a) for i in range(NDW)]
            Bwd = [Buf(f"wd{i}") for i in range(NDW)]
            wdsem = [S.new_sem(f"wd{half}{i}") for i in range(NDW)]
            actT1 = sb("actT", [128, NF, 1024], BF16, p5a)
            actT = [actT1, actT1]
            BactT1 = Buf("actT")
            BactT = [BactT1, BactT1]
            slt = [sb(f"slt{i}", [128, 512], F32, p5a) for i in range(2)]
            Bslt = [Buf(f"slt{i}") for i in range(2)]
            gui = 0
            wdi = 0
            pbi = 0
            sli = 0
            for e_ in range(NE + 1):
                a_i = e_ % 2
                for f in range(NF):
                    r = gui % NGU
                    gui += 1
                    dma_in(gusem[r], wgr[r][:], weg_d[e_, f], [Bgu[r]], queue="pool")
                    dma_in(gusem[r], wur[r][:], weu_d[e_, f], [Bgu[r]], queue="pool")
                    for tg_ in range(2):
                        gb = (pbi % 3) * 2
                        pbi += 1
                        ub = gb + 1
                        tk = slice(tg_ * 512, (tg_ + 1) * 512)
                        mm_group(pb[gb][:], [(wgr[r][:, kc, :], hT2[:, kc, tk]) for kc in range(16)],
                                 [Bgu[r], BhT2], [BP[gb]])
                        mm_group(pb[ub][:], [(wur[r][:, kc, :], hT2[:, kc, tk]) for kc in range(16)],
                                 [Bgu[r], BhT2], [BP[ub]])
                        si = sli % 2
                        sli += 1
                        act(slt[si][:], pb[gb][:], AF.Silu, [BP[gb]], [Bslt[si]])
                        tt("dve", actT[a_i][:, f, tk], slt[si][:], pb[ub][:], ALU.mult, [Bslt[si], BP[ub]], [BactT[a_i]])
                for c8 in range(8):
                    r = wdi % NDW
                    wdi += 1
                    dma_in(wdsem[r], wdr[r][:], wed_d[e_, c8], [Bwd[r]], queue="pool")
                    for tl in range(8):
                        bk = 6 + (tl % 2)
                        mm_group(pb[bk][:, 0:256], [(actT[a_i][:, f, tl * 128:(tl + 1) * 128], wdr[r][:, f, :])
                                                    for f in range(NF)], [BactT[a_i], Bwd[r]], [BP[bk]])
                        dst = acc[:, tl, c8 * 256:(c8 + 1) * 256]
                        if e_ == 0:
                            ts("dve", dst, pb[bk][:, 0:256], G[:, tl, e_:e_ + 1], None, ALU.mult, None,
                               [BP[bk], BG], [Bacc[tl]])
                        else:
                            stt("dve", dst, pb[bk][:, 0:256], G[:, tl, e_:e_ + 1], dst, ALU.mult, ALU.add,
                                [BP[bk], BG, Bacc[tl]], [Bacc[tl]])
            S.barrier()
            p5a.close()
            x1r = [sb(f"x1r{i}", [128, D], F32, p5) for i in range(2)]
            Bx1r = [Buf(f"x1r{i}") for i in range(2)]
            x1sem = [S.new_sem(f"x1r{half}{i}") for i in range(2)]
            fo = [sb(f"fo{i}", [128, D], F32, p5) for i in range(2)]
            Bfo = [Buf(f"fo{i}") for i in range(2)]
            junk5 = sb("junk5", [128, D], BF16, p5)
            Bjunk5 = Buf("junk5")
            st5 = sb("st5", [128, 16], F32, p5)
            Bst5 = Buf("st5")
            for tl in range(8):
                tg_ = half * 8 + tl
                s = tl % 2
                dma_in(x1sem[s], x1r[s][:], x1_s[tg_ * 128:(tg_ + 1) * 128, :], [Bx1r[s]], reads=[Bx1s])
                act(junk5[:], acc[:, tl, :], AF.Square, [Bacc[tl]], [Bjunk5, Bst5], accum_out=st5[:, 2 * s:2 * s + 1])
                rstd_from_ssq(st5[:, 2 * s:2 * s + 1], st5[:, 2 * s + 1:2 * s + 2], Bst5, Bst5, 1.0 / D)
                stt("dve", fo[s][:], acc[:, tl, :], st5[:, 2 * s + 1:2 * s + 2], gg[:, 1, :], ALU.mult, ALU.mult,
                    [Bacc[tl], Bst5, Bgg], [Bfo[s]])
                tt("pool", fo[s][:], fo[s][:], x1r[s][:], ALU.add, [Bfo[s], Bx1r[s]], [Bfo[s]])
                dma_in(st_sem, out_d[tg_ * 128:(tg_ + 1) * 128, :], fo[s][:], [], reads=[Bfo[s]])
            S.barrier()
            p5.close()
        if dbg:
            dma_in(st_sem, dbg_out["d_x1"], x1_s, [], reads=[Bx1s])
        ph4.close()

    S.barrier()

    block = ec(nc.Block())

    @block.sync
    def _(e):
        S.replay("sp", e)

    @block.scalar
    def _(e):
        S.replay("act", e)

    @block.vector
    def _(e):
        S.replay("dve", e)

    @block.gpsimd
    def _(e):
        S.replay("pool", e)

    @block.tensor
    def _(e):
        S.replay("pe", e)

    es.close()
    return nc


def _rope_tables():
    rows = S_LEN // 64
    t = np.arange(S_LEN)
    row = (t // 64).astype(np.float32)
    col = (t % 64).astype(np.float32)
    inv_freq = (np.float32(10000.0) ** (-np.arange(0, 64, 2, dtype=np.float32) / np.float32(64))).astype(np.float32)
    cosT = np.zeros((128, S_LEN), np.float32)
    sinT = np.zeros((128, S_LEN), np.float32)
    perm = np.zeros((128, 128), np.float32)
    for d in range(128):
        axis, r = d // 64, d % 64
        half, i = r // 32, r % 32
        pos = row if axis == 0 else col
        ang = (pos * inv_freq[i]).astype(np.float32)
        cosT[d] = np.cos(ang)
        sinT[d] = -np.sin(ang) if half == 0 else np.sin(ang)
        partner = d + 32 if half == 0 else d - 32
        perm[partner, d] = 1.0
    return cosT, sinT, perm


def _ktile(w, nk=16):
    K, M = w.shape
    return np.ascontiguousarray(w.reshape(K // 128, 128, M).transpose(1, 0, 2))


def _prep_shared(inp, with_experts=True):
    f = np.float32
    w_in = np.asarray(inp["w_in"], f)[0]
    sh = {}
    sh["wmod_l"] = np.ascontiguousarray(np.asarray(inp["w_mod"], f)[0].reshape(16, 128, 24, 512).transpose(2, 1, 0, 3))
    b_mod = np.asarray(inp["b_mod"], f)[0]
    sh["bmodT"] = np.ascontiguousarray(b_mod.reshape(96, 128).T)
    sh["bgt_rep"] = np.ascontiguousarray(np.broadcast_to(
        np.stack([b_mod[4096:6144], b_mod[10240:12288]])[None], (128, 2, D)))
    sh["gpost_rep"] = np.ascontiguousarray(np.broadcast_to(
        np.stack([np.asarray(inp["g_post_mix"], f)[0], np.asarray(inp["g_post_ffn"], f)[0]])[None], (128, 2, D)))
    sh["gpreT"] = np.ascontiguousarray(np.stack([np.asarray(inp["g_pre_mix"], f)[0].reshape(16, 128).T,
                                                 np.asarray(inp["g_pre_ffn"], f)[0].reshape(16, 128).T], axis=1))
    sh["qkg"] = np.ascontiguousarray(np.stack([np.asarray(inp["q_norm_g"], f)[0], np.asarray(inp["k_norm_g"], f)[0]], axis=1))
    sh["wk_l"] = _ktile(w_in[:, 2048:2560])
    sh["wv_l"] = _ktile(w_in[:, 2560:3072])

    def heads(wc):
        return np.ascontiguousarray(wc.reshape(16, 128, 16, 128).transpose(2, 1, 0, 3))
    sh["wq_l"] = heads(w_in[:, 0:2048])
    sh["wp_l"] = heads(w_in[:, 3072:5120])
    sh["wga_l"] = heads(w_in[:, 5120:7168])
    sh["wgp_l"] = heads(w_in[:, 7168:9216])
    sh["wpool_l"] = np.ascontiguousarray(np.asarray(inp["w_pool"], f)[0].reshape(4, 4, 128, 512).transpose(0, 2, 1, 3))
    sh["pscT"] = np.ascontiguousarray(np.asarray(inp["pool_scale"], f)[0].reshape(16, 128).T)
    sh["wout_l"] = _ktile(np.asarray(inp["w_out"], f)[0])
    sh["wr_l"] = _ktile(np.asarray(inp["w_router"], f)[0])
    sh["rbias_rep"] = np.ascontiguousarray(np.broadcast_to(np.asarray(inp["router_bias"], f)[0][None], (128, NE)))

    sh["ident"] = np.eye(128, dtype=f)
    cosT, sinT, perm = _rope_tables()
    sh["perm"] = perm
    sh["cosk"] = cosT
    sh["sink"] = sinT

    def gu(we, ws):
        w = np.concatenate([np.asarray(we, f)[0], np.asarray(ws, f)[0][None]], axis=0)
        return np.ascontiguousarray(w.reshape(NE + 1, 16, 128, NF, 128).transpose(0, 3, 2, 1, 4))
    if not with_experts:
        return sh, cosT, sinT
    sh["weg_l"] = gu(inp["w_exp_gate"], inp["w_sh_gate"])
    sh["weu_l"] = gu(inp["w_exp_up"], inp["w_sh_up"])
    wd = np.concatenate([np.asarray(inp["w_exp_down"], f)[0], np.asarray(inp["w_sh_down"], f)[0][None]], axis=0)
    sh["wed_l"] = np.ascontiguousarray(wd.reshape(NE + 1, NF, 128, 8, 256).transpose(0, 3, 2, 1, 4))
    return sh, cosT, sinT


def _prep_core(inp, sh, cosT, sinT, core):
    f = np.float32
    x = np.asarray(inp["x"], f)
    c = np.asarray(inp["c"], f)
    b, pos = core // 4, core % 4
    t0 = pos * NOWN
    m = dict(sh)
    m["xb"] = x[b]
    xe = np.zeros((NEXT, D), f)
    xe[8:8 + NOWN] = x[b, t0:t0 + NOWN]
    if pos > 0:
        xe[0:8] = x[b, t0 - 8:t0]
    if pos < 3:
        xe[8 + NOWN:] = x[b, t0 + NOWN:t0 + NOWN + 8]
    m["xe"] = xe
    m["cT"] = np.ascontiguousarray(c[b].reshape(16, 128).T)
    m["cosq"] = np.ascontiguousarray(cosT[:, t0:t0 + NOWN])
    m["sinq"] = np.ascontiguousarray(sinT[:, t0:t0 + NOWN])
    m["hflag"] = np.ascontiguousarray(np.broadcast_to(np.array([pos > 0, pos < 3], f)[None], (128, 2)))
    ie = np.zeros((4, 16), f)
    for wi, w in enumerate((2, 4, 8, 16)):
        for j in range(16):
            t = t0 + j if j < 8 else t0 + NOWN - 8 + (j - 8)
            lo = max(t - w // 2, 0)
            hi = min(t - w // 2 + w, S_LEN)
            ie[wi, j] = 1.0 / float(hi - lo)
    m["iedge"] = np.ascontiguousarray(np.broadcast_to(ie[None], (128, 4, 16)))
    return m


def kernel(**inputs):
    import time
    _t0 = time.time()
    stop_after = DEBUG.get("stop_after", 99)
    sh, cosT, sinT = _prep_shared(inputs, with_experts=stop_after >= 5)
    in_maps = [_prep_core(inputs, sh, cosT, sinT, core) for core in range(8)]
    dbg = DEBUG.get("dbg", False)
    _t1 = time.time()
    nc = build_program(stop_after=stop_after, dbg=dbg)
    _t2 = time.time()
    ncores = DEBUG.get("ncores", 8)
    res = run_bass_kernel_spmd(nc, in_maps[:ncores], core_ids=list(range(ncores)))
    print(f"[kernel] prep {_t1 - _t0:.1f}s build {_t2 - _t1:.1f}s run {time.time() - _t2:.1f}s", flush=True)
    DEBUG["res"] = res
    out = np.zeros((2, S_LEN, D), np.float32)
    for core in range(ncores):
        b, pos = core // 4, core % 4
        out[b, pos * NOWN:(pos + 1) * NOWN] = np.asarray(res.results[core]["out"], np.float32)
    return out
```

```python
import numpy as np
from contextlib import ExitStack
import concourse.bass as bass
import concourse.mybir as mybir
from concourse.bass_utils import run_bass_kernel_spmd

F32 = mybir.dt.float32
BF16 = mybir.dt.bfloat16
AF = mybir.ActivationFunctionType
ALU = mybir.AluOpType
AX = mybir.AxisListType

D = 2048
S_LEN = 8192
NOWN = 2048
NEXT = NOWN + 16
FF = 1408
NF = 11
NE = 64
EPS = 1e-6
ENG_NAMES = ("pe", "act", "dve", "pool", "sp")

DEBUG = {}


class Buf:
    __slots__ = ("name", "w", "r")

    def __init__(self, name):
        self.name = name
        self.w = None
        self.r = []


class Sched:
    def __init__(self, nc, ec):
        self.nc = nc
        self.ec = ec
        self.streams = {e: [] for e in ENG_NAMES}
        self.cnt = {}
        self.sems = {}
        self.waited = {e: {} for e in ENG_NAMES}
        for e in ("pe", "act", "dve", "pool"):
            self.new_sem(e)

    def new_sem(self, key):
        self.sems[key] = self.ec(self.nc.semaphore(key))
        self.cnt[key] = 0
        return key

    def _waits(self, eng, deps):
        need = {}
        for tok in deps:
            if tok is None:
                continue
            k, v = tok
            if k == "pe" and eng == "pe":
                continue
            if v > need.get(k, 0):
                need[k] = v
        out = []
        wd = self.waited[eng]
        for k, v in need.items():
            if wd.get(k, 0) >= v:
                continue
            wd[k] = v
            out.append((k, v))
        return out

    @staticmethod
    def _deps(reads, writes):
        deps = []
        for b in reads:
            if b.w is not None:
                deps.append(b.w)
        for b in writes:
            if b.w is not None:
                deps.append(b.w)
            deps.extend(b.r)
        return deps

    @staticmethod
    def _commit(tok, reads, writes):
        for b in reads:
            b.r.append(tok)
            if len(b.r) > 64:
                best = {}
                for k, v in b.r:
                    if v > best.get(k, 0):
                        best[k] = v
                b.r = list(best.items())
        for b in writes:
            b.w = tok
            b.r = []

    def op(self, eng, emit, reads=(), writes=()):
        waits = self._waits(eng, self._deps(reads, writes))
        self.cnt[eng] += 1
        tok = (eng, self.cnt[eng])
        self.streams[eng].append((waits, emit, (eng, 1)))
        self._commit(tok, reads, writes)
        return tok

    def dma(self, queue, semkey, emit, reads=(), writes=()):
        waits = self._waits(queue, self._deps(reads, writes))
        self.cnt[semkey] += 16
        tok = (semkey, self.cnt[semkey])
        self.streams[queue].append((waits, emit, (semkey, 16)))
        self._commit(tok, reads, writes)
        return tok

    def barrier(self):
        allv = [(k, v) for k, v in self.cnt.items() if v > 0]
        for e in ENG_NAMES:
            waits = self._waits(e, allv)
            if waits:
                self.streams[e].append((waits, None, None))

    def replay(self, eng, eobj):
        sems = self.sems
        for waits, emit, inc in self.streams[eng]:
            for k, v in waits:
                eobj.wait_ge(sems[k], v)
            if emit is not None:
                ins = emit(eobj)
                ins.then_inc(sems[inc[0]], inc[1])


def build_program(stop_after=99, dbg=False):
    nc = bass.Bass("TRN2", target_bir_lowering=False)
    es = ExitStack()
    ec = es.enter_context

    def din(name, shape, dt=F32):
        return nc.dram_tensor(name, list(shape), dt, kind="ExternalInput").ap()

    xb = din("xb", [S_LEN, D])
    xe = din("xe", [NEXT, D])
    cT_d = din("cT", [128, 16])
    wmod_d = din("wmod_l", [24, 128, 16, 512])
    bmodT_d = din("bmodT", [128, 96])
    bgt_d = din("bgt_rep", [128, 2, D])
    gpost_d = din("gpost_rep", [128, 2, D])
    gpre_d = din("gpreT", [128, 2, 16])
    qkg_d = din("qkg", [128, 2])
    wk_d = din("wk_l", [128, 16, 512])
    wv_d = din("wv_l", [128, 16, 512])
    wq_d = din("wq_l", [16, 128, 16, 128])
    wp_d = din("wp_l", [16, 128, 16, 128])
    wga_d = din("wga_l", [16, 128, 16, 128])
    wgp_d = din("wgp_l", [16, 128, 16, 128])
    wpool_d = din("wpool_l", [4, 128, 4, 512])
    pscT_d = din("pscT", [128, 16])
    wout_d = din("wout_l", [128, 16, D])
    wr_d = din("wr_l", [128, 16, NE])
    rbias_d = din("rbias_rep", [128, NE])
    if stop_after >= 5:
        weg_d = din("weg_l", [NE + 1, NF, 128, 16, 128])
        weu_d = din("weu_l", [NE + 1, NF, 128, 16, 128])
        wed_d = din("wed_l", [NE + 1, 8, 128, NF, 256])
    ident_d = din("ident", [128, 128])
    perm_d = din("perm", [128, 128])
    cosk_d = din("cosk", [128, S_LEN])
    sink_d = din("sink", [128, S_LEN])
    cosq_d = din("cosq", [128, NOWN])
    sinq_d = din("sinq", [128, NOWN])
    hflag_d = din("hflag", [128, 2])
    iedge_d = din("iedge", [128, 4, 16])
    out_d = nc.dram_tensor("out", [NOWN, D], F32, kind="ExternalOutput").ap()
    kT_s = nc.dram_tensor("kT_s", [4, 128, S_LEN], BF16).ap()
    v_s = nc.dram_tensor("v_s", [4, 128, 64 * 128], BF16).ap()
    mrg_s = nc.dram_tensor("mrg_s", [16, 128, NOWN], BF16).ap()
    x1_s = nc.dram_tensor("x1_s", [NOWN, D], F32).ap()
    dbg_out = {}
    if dbg:
        dbg_out["d_hT"] = nc.dram_tensor("d_hT", [128, 16, NEXT], BF16, kind="ExternalOutput").ap()
        dbg_out["d_G"] = nc.dram_tensor("d_G", [128, 16, NE], F32, kind="ExternalOutput").ap()
        dbg_out["d_hT2"] = nc.dram_tensor("d_hT2", [128, 16, NOWN], BF16, kind="ExternalOutput").ap()
        dbg_out["d_kT"] = nc.dram_tensor("d_kT", [4, 128, S_LEN], BF16, kind="ExternalOutput").ap()
        dbg_out["d_v"] = nc.dram_tensor("d_v", [4, 128, 64 * 128], BF16, kind="ExternalOutput").ap()
        dbg_out["d_mrg"] = nc.dram_tensor("d_mrg", [16, 128, NOWN], BF16, kind="ExternalOutput").ap()
        dbg_out["d_x1"] = nc.dram_tensor("d_x1", [NOWN, D], F32, kind="ExternalOutput").ap()

    S = Sched(nc, ec)
    block = None

    sbc = [0]

    def sb(name, shape, dt, stack=None):
        sbc[0] += 1
        return (stack or es).enter_context(nc.sbuf_tensor(f"{name}_{sbc[0]}", list(shape), dt))

    pb = [ec(nc.psum_tensor(f"pb{i}", [128, 512], F32)) for i in range(8)]
    BP = [Buf(f"pb{i}") for i in range(8)]

    ident = sb("ident", [128, 128], F32)
    identb = sb("identb", [128, 128], BF16)
    permb = sb("permb", [128, 128], BF16)
    onesb = sb("onesb", [128, 128], BF16)
    ones32 = sb("ones32", [128, 128], F32)
    epsc = sb("epsc", [128, 1], F32)
    cT = sb("cT", [128, 16], F32)
    modT = sb("modT", [128, 96], F32)
    gpre = sb("gpre", [128, 2, 16], F32)
    A_a = sb("A_a", [128, 16], F32)
    A_f = sb("A_f", [128, 16], F32)
    qkg = sb("qkg", [128, 2], F32)
    pscT = sb("pscT", [128, 16], F32)
    hflag = sb("hflag", [128, 2], F32)
    iedge = sb("iedge", [128, 4, 16], F32)
    gg = sb("gg", [128, 2, D], F32)
    rbias = sb("rbias", [128, NE], F32)
    Bconst = Buf("const")
    Bmod = Buf("mod")
    Bgg = Buf("gg")

    ldc = S.new_sem("ldc")
    st_sem = S.new_sem("st")

    def dma_in(sem, out_ap, in_ap, writes, reads=(), queue="sp"):
        return S.dma(queue, sem, lambda e: e.dma_start(out=out_ap, in_=in_ap), reads=reads, writes=writes)

    def mm_group(out_ap, pairs, reads, writes):
        def emit(e):
            n = len(pairs)
            ins = None
            for i, (l, r) in enumerate(pairs):
                ins = e.matmul(out_ap, lhsT=l, rhs=r, start=(i == 0), stop=(i == n - 1))
            return ins
        return S.op("pe", emit, reads, writes)

    def act(out, in_, func, reads, writes, **kw):
        return S.op("act", lambda e: e.activation(out=out, in_=in_, func=func, **kw), reads, writes)

    def ts(eng, out, in0, s1, s2, op0, op1, reads, writes):
        if op1 is None:
            return S.op(eng, lambda e: e.tensor_scalar(out=out, in0=in0, scalar1=s1, scalar2=None, op0=op0), reads, writes)
        return S.op(eng, lambda e: e.tensor_scalar(out=out, in0=in0, scalar1=s1, scalar2=s2, op0=op0, op1=op1), reads, writes)

    def tt(eng, out, in0, in1, op, reads, writes):
        return S.op(eng, lambda e: e.tensor_tensor(out=out, in0=in0, in1=in1, op=op), reads, writes)

    def stt(eng, out, in0, scalar, in1, op0, op1, reads, writes):
        return S.op(eng, lambda e: e.scalar_tensor_tensor(out=out, in0=in0, scalar=scalar, in1=in1, op0=op0, op1=op1),
                    reads, writes)

    def cp(eng, out, in_, reads, writes):
        return S.op(eng, lambda e: e.tensor_copy(out=out, in_=in_), reads, writes)

    def rstd_from_ssq(ssq_ap, out_ap, Bssq, Bout, inv_n):
        act(out_ap, ssq_ap, AF.Sqrt, [Bssq, Bconst], [Bout], bias=epsc[:, 0:1], scale=inv_n)
        S.op("dve", lambda e: e.reciprocal(out=out_ap, in_=out_ap), [Bout], [Bout])

    ph0 = ExitStack()
    for (t, d_) in ((ident, ident_d), (cT, cT_d), (modT, bmodT_d), (gpre, gpre_d), (qkg, qkg_d), (pscT, pscT_d),
                    (hflag, hflag_d), (iedge, iedge_d), (rbias, rbias_d)):
        dma_in(ldc, t[:], d_, [Bconst])
    perm32 = sb("perm32", [128, 128], F32, ph0)
    dma_in(ldc, perm32[:], perm_d, [Bconst])
    bgt = sb("bgt", [128, 2, D], F32, ph0)
    gpo = sb("gpo", [128, 2, D], F32, ph0)
    dma_in(ldc, bgt[:], bgt_d, [Bconst])
    dma_in(ldc, gpo[:], gpost_d, [Bconst])
    Bconst.w = (ldc, S.cnt[ldc])
    S.op("dve", lambda e: e.memset(onesb[:], 1.0), [], [Bconst])
    S.op("dve", lambda e: e.memset(ones32[:], 1.0), [], [Bconst])
    S.op("dve", lambda e: e.memset(epsc[:], EPS), [], [Bconst])
    cp("dve", identb[:], ident[:], [Bconst], [Bconst])
    cp("dve", permb[:], perm32[:], [Bconst], [Bconst])
    act(cT[:], cT[:], AF.Silu, [Bconst], [Bconst])
    cTb = sb("cTb", [128, 16, 128], F32, ph0)
    for kc in range(16):
        act(cTb[:, kc, :], ones32[:], AF.Copy, [Bconst], [Bconst], scale=cT[:, kc:kc + 1])
    wm = [sb(f"wm{i}", [128, 16, 512], F32, ph0) for i in range(2)]
    Bwm = [Buf(f"wm{i}") for i in range(2)]
    wm_sem = [S.new_sem(f"wm{i}") for i in range(2)]
    Bpm = BP[0]
    first_fm = True
    fm_blocks = list(range(0, 8)) + list(range(12, 20))
    for blk in range(24):
        s = blk % 2
        dma_in(wm_sem[s], wm[s][:], wmod_d[blk], [Bwm[s]])
        if blk in fm_blocks:
            for jj in range(4):
                j = blk * 4 + jj
                pairs = [(wm[s][:, kc, jj * 128:(jj + 1) * 128], cT[:, kc:kc + 1]) for kc in range(16)]
                mm_group(pb[0][:, j:j + 1], pairs, [Bwm[s], Bconst], [Bpm])
        else:
            gi = 0 if blk < 12 else 1
            cb = (blk - 8) if blk < 12 else (blk - 20)
            pairs = [(cTb[:, kc, :], wm[s][:, kc, :]) for kc in range(16)]
            bank = 1 + (blk % 2)
            mm_group(pb[bank][:], pairs, [Bwm[s], Bconst], [BP[bank]])
            sl = slice(cb * 512, (cb + 1) * 512)
            tt("dve", gg[:, gi, sl], pb[bank][:], bgt[:, gi, sl], ALU.add, [BP[bank], Bconst], [Bgg])
            tt("dve", gg[:, gi, sl], gg[:, gi, sl], gpo[:, gi, sl], ALU.mult, [Bgg, Bconst], [Bgg])
    for (a, b_) in ((0, 32), (48, 80)):
        tt("dve", modT[:, a:b_], pb[0][:, a:b_], modT[:, a:b_], ALU.add, [Bpm, Bconst], [Bmod])
    stt("dve", A_a[:], modT[:, 16:32], 1.0, gpre[:, 0, :], ALU.add, ALU.mult, [Bmod, Bconst], [Bmod])
    stt("dve", A_f[:], modT[:, 64:80], 1.0, gpre[:, 1, :], ALU.add, ALU.mult, [Bmod, Bconst], [Bmod])
    sh_a = modT[:, 0:16]
    sh_f = modT[:, 48:64]
    S.barrier()
    ph0.close()

    def norm_transpose_group(stack_bufs, x_src_ap, dst_fn, Bdst, Aap, shap, tbanks):
        (xt, Bxt, xsem, ssq, Bssq, xn, Bxn, junk, Bjunk) = stack_bufs
        dma_in(xsem, xt[:], x_src_ap, [Bxt])
        for t in range(4):
            act(junk[:], xt[:, t, :], AF.Square, [Bxt], [Bjunk, Bssq], accum_out=ssq[:, t:t + 1])
        rstd_from_ssq(ssq[:, 0:4], ssq[:, 4:8], Bssq, Bssq, 1.0 / D)
        for t in range(4):
            if t % 2 == 0:
                ts("dve", xn[:, t, :], xt[:, t, :], ssq[:, 4 + t:5 + t], None, ALU.mult, None, [Bxt, Bssq], [Bxn])
            else:
                act(xn[:, t, :], xt[:, t, :], AF.Copy, [Bxt, Bssq], [Bxn], scale=ssq[:, 4 + t:5 + t])
        for c in range(16):
            bk = tbanks[c % 2]

            def emit(e, c=c, bk=bk):
                ins = None
                for t in range(4):
                    ins = e.transpose(pb[bk][:, t * 128:(t + 1) * 128], xn[:, t, c * 128:(c + 1) * 128], ident[:])
                return ins
            S.op("pe", emit, [Bxn, Bconst], [BP[bk]])
            act(dst_fn(c), pb[bk][:], AF.Identity, [BP[bk], Bmod], [Bdst], bias=shap[:, c:c + 1], scale=Aap[:, c:c + 1])

    def qk_post(psrc_bank, gcol, cos_ap, sin_ap, Btab, out_ap, Bout, tmp, Btmp, abank, rbank):
        (sq, tg, u, w, rs) = tmp
        ps = pb[psrc_bank]
        lim = DEBUG.get("qkstage", 99)
        if lim <= 0:
            return
        act(sq[:], ps[:], AF.Copy, [BP[psrc_bank]], [Btmp[0]])
        stt("dve", sq[:], ps[:], 1.0, sq[:], ALU.mult, ALU.mult, [BP[psrc_bank], Btmp[0]], [Btmp[0]])
        if lim <= 1:
            return
        ts("dve", tg[:], ps[:], qkg[:, gcol:gcol + 1], None, ALU.mult, None, [BP[psrc_bank], Bconst], [Btmp[1]])
        if lim <= 2:
            return
        mm_group(pb[abank][:], [(onesb[:], sq[:])], [Btmp[0], Bconst], [BP[abank]])
        if lim <= 3:
            return
        mm_group(pb[rbank][:], [(permb[:], tg[:])], [Btmp[1], Bconst], [BP[rbank]])
        if lim <= 4:
            return
        act(rs[:], pb[abank][:], AF.Sqrt, [BP[abank], Bconst], [Btmp[4]], bias=epsc[:, 0:1], scale=1.0 / 128)
        if lim <= 5:
            return
        S.op("dve", lambda e: e.reciprocal(out=rs[:], in_=rs[:]), [Btmp[4]], [Btmp[4]])
        if lim <= 6:
            return
        tt(DEBUG.get("qkeng", "pool"), u[:], tg[:], cos_ap, ALU.mult, [Btmp[1], Btab], [Btmp[2]])
        if lim <= 7:
            return
        tt("dve", w[:], pb[rbank][:], sin_ap, ALU.mult, [BP[rbank], Btab], [Btmp[3]])
        if lim <= 8:
            return
        tt(DEBUG.get("qkeng", "pool"), u[:], u[:], w[:], ALU.add, [Btmp[2], Btmp[3]], [Btmp[2]])
        if lim <= 9:
            return
        tt("dve", out_ap, u[:], rs[:], ALU.mult, [Btmp[2], Btmp[4]], [Bout])

    ph1 = ExitStack()
    xt1 = sb("xt", [128, 4, D], F32, ph1)
    xt = [xt1, xt1]
    Bxt1 = Buf("xt")
    Bxt = [Bxt1, Bxt1]
    xs1 = S.new_sem("xs")
    xsem = [xs1, xs1]
    ssq = [sb(f"ssq{i}", [128, 8], F32, ph1) for i in range(2)]
    Bssq = [Buf(f"ssq{i}") for i in range(2)]
    xn = sb("xn", [128, 4, D], F32, ph1)
    Bxn = Buf("xn")
    junk = sb("junk", [128, D], BF16, ph1)
    Bjunk = Buf("junk")
    hTg = sb("hTg", [128, 16, 512], BF16, ph1)
    BhTg = Buf("hTg")
    wk = sb("wk", [128, 16, 512], BF16, ph1)
    wv = sb("wv", [128, 16, 512], BF16, ph1)
    Bwkv = Buf("wkv")
    wsem = S.new_sem("wkv")
    dma_in(wsem, wk[:], wk_d, [Bwkv], queue="pool")
    dma_in(wsem, wv[:], wv_d, [Bwkv], queue="pool")
    ctab = [sb(f"ctab{i}", [128, 2, 512], F32, ph1) for i in range(2)]
    Bctab = [Buf(f"ctab{i}") for i in range(2)]
    csem = [S.new_sem(f"cs{i}") for i in range(2)]
    qtmp = (sb("q_sq", [128, 512], BF16, ph1), sb("q_tg", [128, 512], BF16, ph1), sb("q_u", [128, 512], F32, ph1),
            sb("q_w", [128, 512], F32, ph1), sb("q_rs", [128, 512], F32, ph1))
    Bqtmp = [Buf(f"qtmp{i}") for i in range(5)]
    kTg = [sb(f"kTg{i}", [128, 4, 512], BF16, ph1) for i in range(2)]
    BkTg = [Buf(f"kTg{i}") for i in range(2)]
    vg = [sb(f"vg{i}", [128, 4, 4, 128], BF16, ph1) for i in range(2)]
    Bvg = [Buf(f"vg{i}") for i in range(2)]
    kv_st = S.new_sem("kvst")
    Bks = Buf("kT_s")
    Bvs = Buf("v_s")

    for g in range(DEBUG.get('kv_groups', 16) if stop_after >= 1 else 0):
        s = g % 2
        dma_in(csem[s], ctab[s][:, 0, :], cosk_d[:, g * 512:(g + 1) * 512], [Bctab[s]])
        dma_in(csem[s], ctab[s][:, 1, :], sink_d[:, g * 512:(g + 1) * 512], [Bctab[s]])
        norm_transpose_group((xt[s], Bxt[s], xsem[s], ssq[s], Bssq[s], xn, Bxn, junk, Bjunk),
                             xb[g * 512:(g + 1) * 512, :].rearrange("(t p) d -> p t d", p=128),
                             lambda c: hTg[:, c, :], BhTg, A_a, sh_a, (0, 1))
        def k_mm(hd):
            bk = 2 + (hd % 2)
            mm_group(pb[bk][:], [(wk[:, kc, hd * 128:(hd + 1) * 128], hTg[:, kc, :]) for kc in range(16)],
                     [Bwkv, BhTg], [BP[bk]])

        def k_post(hd):
            qk_post(2 + (hd % 2), 1, ctab[s][:, 0, :], ctab[s][:, 1, :], Bctab[s], kTg[s][:, hd, :], BkTg[s],
                    qtmp, Bqtmp, 6, 7)

        def v_mm(t):
            bk = 4 + (t % 2)
            mm_group(pb[bk][:], [(hTg[:, kc, t * 128:(t + 1) * 128], wv[:, kc, :]) for kc in range(16)],
                     [Bwkv, BhTg], [BP[bk]])
            act(vg[s][:, :, t, :], pb[bk][:].rearrange("p (h d) -> p h d", h=4), AF.Copy, [BP[bk]], [Bvg[s]])
        for half in range(2):
            k_mm(2 * half)
            k_mm(2 * half + 1)
            v_mm(2 * half)
            v_mm(2 * half + 1)
            k_post(2 * half)
            k_post(2 * half + 1)
        for hd in range(0 if DEBUG.get("nostore") else 4):
            dma_in(kv_st, kT_s[hd, :, g * 512:(g + 1) * 512], kTg[s][:, hd, :], [Bks], reads=[BkTg[s]])
            dma_in(kv_st, v_s[hd, :, g * 512:(g + 1) * 512], vg[s][:, hd, :, :].rearrange("p t d -> p (t d)"),
                   [Bvs], reads=[Bvg[s]])

    if dbg:
        dma_in(st_sem, dbg_out["d_kT"], kT_s, [], reads=[Bks])
        dma_in(st_sem, dbg_out["d_v"], v_s, [], reads=[Bvs])
    S.barrier()
    ph1.close()
    ph23 = ExitStack()
    hT = sb("hT", [128, 16, NEXT], BF16, ph23)
    BhT = Buf("hT")
    ph1 = ExitStack()
    xt1 = sb("xt", [128, 4, D], F32, ph1)
    xt = [xt1, xt1]
    Bxt1 = Buf("xt")
    Bxt = [Bxt1, Bxt1]
    ssq = [sb(f"ssq{i}", [128, 8], F32, ph1) for i in range(2)]
    Bssq = [Buf(f"ssq{i}") for i in range(2)]
    xn = sb("xn", [128, 4, D], F32, ph1)
    Bxn = Buf("xn")
    junk = sb("junk", [128, D], BF16, ph1)
    Bjunk = Buf("junk")
    for g in range(4 if stop_after >= 2 else 0):
        s = g % 2
        norm_transpose_group((xt[s], Bxt[s], xsem[s], ssq[s], Bssq[s], xn, Bxn, junk, Bjunk),
                             xe[8 + g * 512:8 + (g + 1) * 512, :].rearrange("(t p) d -> p t d", p=128),
                             lambda c, g=g: hT[:, c, 8 + g * 512:8 + (g + 1) * 512], BhT, A_a, sh_a, (0, 1))
    xh = sb("xh", [16, D], F32, ph1)
    Bxh = Buf("xh")
    hsem = S.new_sem("halo")
    dma_in(hsem, xh[0:8, :], xe[0:8, :], [Bxh])
    dma_in(hsem, xh[8:16, :], xe[8 + NOWN:16 + NOWN, :], [Bxh])
    sqh = sb("sqh", [16, 2], F32, ph1)
    Bsqh = Buf("sqh")
    xnh = sb("xnh", [16, D], F32, ph1)
    act(junk[0:16, :], xh[:], AF.Square, [Bxh], [Bjunk, Bsqh], accum_out=sqh[:, 0:1])
    act(sqh[:, 1:2], sqh[:, 0:1], AF.Sqrt, [Bsqh, Bconst], [Bsqh], bias=epsc[0:16, 0:1], scale=1.0 / D)
    S.op("dve", lambda e: e.reciprocal(out=sqh[:, 1:2], in_=sqh[:, 1:2]), [Bsqh], [Bsqh])
    ts("dve", xnh[:], xh[:], sqh[:, 1:2], None, ALU.mult, None, [Bxh, Bsqh], [Bxh])

    def emit_h(e):
        ins = None
        for c in range(16):
            ins = e.transpose(pb[0][:, c * 16:(c + 1) * 16], xnh[:, c * 128:(c + 1) * 128], ident[0:16, 0:16])
        return ins
    S.op("pe", emit_h, [Bxh, Bconst], [BP[0]])
    htmp = sb("htmp", [128, 16, 16], F32, ph1)
    Bhtmp = Buf("htmp")
    for c in range(16):
        act(htmp[:, c, :], pb[0][:, c * 16:(c + 1) * 16], AF.Identity, [BP[0], Bmod], [Bhtmp],
            bias=sh_a[:, c:c + 1], scale=A_a[:, c:c + 1])
    ts("dve", hT[:, :, 0:8], htmp[:, :, 0:8], hflag[:, 0:1], None, ALU.mult, None, [Bhtmp, Bconst], [BhT])
    ts("dve", hT[:, :, 8 + NOWN:16 + NOWN], htmp[:, :, 8:16], hflag[:, 1:2], None, ALU.mult, None, [Bhtmp, Bconst], [BhT])
    if dbg:
        dma_in(st_sem, dbg_out["d_hT"], hT[:], [], reads=[BhT])
    S.barrier()
    ph1.close()

    if stop_after >= 3:
        ph3 = ExitStack()
        kTh = sb("kTh", [128, S_LEN], BF16, ph3)
        vh = sb("vh", [128, 64, 128], BF16, ph3)
        Bkv = Buf("kvh")
        kvsem = S.new_sem("kvld")
        cq = sb("cq", [128, 2, NOWN], F32, ph3)
        Bcq = Buf("cq")
        dma_in(ldc, cq[:, 0, :], cosq_d, [Bcq])
        dma_in(ldc, cq[:, 1, :], sinq_d, [Bcq])
        qT = sb("qT", [128, NOWN], BF16, ph3)
        BqT = Buf("qT")
        wsl = [sb(f"wsl{i}", [128, 16, 128], BF16, ph3) for i in range(3)]
        Bwsl = [Buf(f"wsl{i}") for i in range(3)]
        wslsem = [S.new_sem(f"wsl{i}") for i in range(3)]
        wslc = [0]

        def load_w(src):
            i = wslc[0] % 3
            wslc[0] += 1
            dma_in(wslsem[i], wsl[i][:], src, [Bwsl[i]], queue="pool")
            return wsl[i], Bwsl[i]
        pT = [sb(f"pT{i}", [128, 512], BF16, ph3) for i in range(4)]
        BpT = [Buf(f"pT{i}") for i in range(4)]
        wpl = sb("wpl", [128, 4, 512], BF16, ph3)
        Bwpl = Buf("wpl")
        wplsem = S.new_sem("wpl")
        pooled = sb("pooled", [128, 4, NOWN], BF16, ph3)
        Bpooled = Buf("pooled")
        mrgh = [sb(f"mrgh{i}", [128, NOWN], BF16, ph3) for i in range(2)]
        Bmrgh = [Buf(f"mrgh{i}") for i in range(2)]
        dacc = [sb(f"dacc{i}", [128, 512], F32, ph3) for i in range(2)]
        Bdacc = [Buf(f"dacc{i}") for i in range(2)]
        phB = None
        mst = S.new_sem("mst")
        Bmrgs = Buf("mrg_s")
        ATT_SCALE = 128.0 ** -0.5
        pti = 0
        for h in range(16):
            kvh, gi = h // 4, h // 4
            if h % 4 == 0:
                dma_in(kvsem, kTh[:], kT_s[kvh], [Bkv], reads=[Bks])
                dma_in(kvsem, vh[:].rearrange("p t d -> p (t d)"), v_s[kvh], [Bkv], reads=[Bvs])
                if phB is not None:
                    S.barrier()
                    phB.close()
                phA = ExitStack()
                pc = sb("pc", [128, NEXT], F32, phA)
                Bpc = Buf("pc")
                sA = sb("sA", [128, NEXT], F32, phA)
                sBt = sb("sBt", [128, NEXT], F32, phA)
                BsA = Buf("sA")
                BsB = Buf("sB")
                dma_in(wplsem, wpl[:], wpool_d[gi], [Bwpl], queue="pool")
                w = (2, 4, 8, 16)[gi]
                for cc in range(4):
                    wp_t, Bwp = load_w(wp_d[gi * 4 + cc])
                    col = 0
                    ci = 0
                    while col < NEXT:
                        n = min(512, NEXT - col)
                        bk = 6 + (ci % 2)
                        mm_group(pb[bk][:, 0:n], [(wp_t[:, kc, :], hT[:, kc, col:col + n]) for kc in range(16)],
                                 [Bwp, BhT], [BP[bk]])
                        act(pc[:, col:col + n], pb[bk][:, 0:n], AF.Copy, [BP[bk]], [Bpc])
                        col += n
                        ci += 1
                    tt("dve", sA[:, 1:NEXT], pc[:, 1:NEXT], pc[:, 0:NEXT - 1], ALU.add, [Bpc], [BsA])
                    cur, Bcur, oth, Both = sA, BsA, sBt, BsB
                    lo, hi = 1, NEXT
                    sh = 1
                    ww = 2
                    while ww < w:
                        nlo, nhi = lo + sh, hi - sh
                        tt("pool", oth[:, nlo:nhi], cur[:, nlo - sh:nhi - sh], cur[:, nlo + sh:nhi + sh], ALU.add,
                           [Bcur], [Both])
                        cur, Bcur, oth, Both = oth, Both, cur, Bcur
                        lo, hi = nlo, nhi
                        sh *= 2
                        ww *= 2
                    stt("dve", pooled[:, cc, :], cur[:, 8:8 + NOWN], 1.0 / w, pc[:, 8:8 + NOWN], ALU.mult, ALU.subtract,
                        [Bcur, Bpc], [Bpooled])
                    for (c0, e0) in ((0, 0), (NOWN - 8, 8)):
                        tt("dve", oth[:, 0:8], cur[:, 8 + c0:16 + c0], iedge[:, gi, e0:e0 + 8], ALU.mult,
                           [Bcur, Bconst], [Both])
                        tt("dve", pooled[:, cc, c0:c0 + 8], oth[:, 0:8], pc[:, 8 + c0:16 + c0], ALU.subtract,
                           [Both, Bpc], [Bpooled])
                S.barrier()
                phA.close()
                phB = ExitStack()
                qtmp = (sb("q_sq", [128, 512], BF16, phB), sb("q_tg", [128, 512], BF16, phB),
                        sb("q_u", [128, 512], F32, phB), sb("q_w", [128, 512], F32, phB), sb("q_rs", [128, 512], F32, phB))
                Bqtmp = [Buf(f"qtmp{i}") for i in range(5)]
                oT = sb("oT", [128, 512], F32, phB)
                BoT = Buf("oT")
                rden = sb("rden", [128, 512], F32, phB)
                Brden = Buf("rden")
                sga = sb("sga", [128, 512], F32, phB)
                sgp = sb("sgp", [128, 512], F32, phB)
                Bsga = Buf("sga")
                Bsgp = Buf("sgp")
            wq_t, Bwq = load_w(wq_d[h])
            wga_t, Bwga = load_w(wga_d[h])
            wgp_t, Bwgp = load_w(wgp_d[h])
            ms = h % 2
            for qg in range(4):
                bk = 6
                mm_group(pb[bk][:], [(wq_t[:, kc, :], hT[:, kc, 8 + qg * 512:8 + (qg + 1) * 512]) for kc in range(16)],
                         [Bwq, BhT], [BP[bk]])
                qk_post(bk, 0, cq[:, 0, qg * 512:(qg + 1) * 512], cq[:, 1, qg * 512:(qg + 1) * 512], Bcq,
                        qT[:, qg * 512:(qg + 1) * 512], BqT, qtmp, Bqtmp, 7, 6)
            for qg in range(4):
                ob, db = (2, 3) if qg % 2 == 0 else (4, 5)
                qs = qT[:, qg * 512:(qg + 1) * 512]
                sbanks = (0, 1, 7)

                def emit_s(kt, qs=qs):
                    sbk = sbanks[kt % 3]
                    mm_group(pb[sbk][:], [(kTh[:, kt * 128:(kt + 1) * 128], qs)], [Bkv, BqT], [BP[sbk]])
                emit_s(0)
                emit_s(1)
                for kt in range(64):
                    sbk = sbanks[kt % 3]
                    p_i = pti % 4
                    pti += 1
                    act(pT[p_i][:], pb[sbk][:], AF.Exp, [BP[sbk]], [BpT[p_i]], scale=ATT_SCALE)
                    if kt + 2 < 64:
                        emit_s(kt + 2)

                    def emit(e, kt=kt, p_i=p_i, ob=ob, db=db):
                        e.matmul(pb[ob][:], lhsT=vh[:, kt, :], rhs=pT[p_i][:], start=(kt == 0), stop=(kt == 63))
                        return e.matmul(pb[db][:], lhsT=onesb[:], rhs=pT[p_i][:], start=(kt == 0), stop=(kt == 63))
                    S.op("pe", emit, [Bkv, BpT[p_i], Bconst], [BP[ob], BP[db]])
                S.op("dve", lambda e, db=db, rden=rden: e.reciprocal(out=rden[:], in_=pb[db][:]), [BP[db]], [Brden])
                tt("dve", oT[:], pb[ob][:], rden[:], ALU.mult, [BP[ob], Brden], [BoT])
                cols = slice(8 + qg * 512, 8 + (qg + 1) * 512)
                oc = slice(qg * 512, (qg + 1) * 512)
                mm_group(pb[6][:], [(wga_t[:, kc, :], hT[:, kc, cols]) for kc in range(16)], [Bwga, BhT], [BP[6]])
                act(sga[:], pb[6][:], AF.Sigmoid, [BP[6]], [Bsga])
                mm_group(pb[7][:], [(wgp_t[:, kc, :], hT[:, kc, cols]) for kc in range(16)], [Bwgp, BhT], [BP[7]])
                act(sgp[:], pb[7][:], AF.Sigmoid, [BP[7]], [Bsgp])
                cc = h % 4
                mm_group(pb[6][:], [(wpl[:, kc, cc * 128:(cc + 1) * 128], pooled[:, kc, oc]) for kc in range(4)],
                         [Bwpl, Bpooled], [BP[6]])
                stt("dve", sgp[:], pb[6][:], pscT[:, h:h + 1], sgp[:], ALU.mult, ALU.mult, [BP[6], Bconst, Bsgp], [Bsgp])
                tt("pool", sga[:], sga[:], oT[:], ALU.mult, [Bsga, BoT], [Bsga])
                tt("dve", mrgh[ms][:, oc], sga[:], sgp[:], ALU.add, [Bsga, Bsgp], [Bmrgh[ms]])
            dma_in(mst, mrg_s[h], mrgh[ms][:], [Bmrgs], reads=[Bmrgh[ms]])
        if dbg:
            dma_in(st_sem, dbg_out["d_mrg"], mrg_s, [], reads=[Bmrgs])
        S.barrier()
        phB.close()
        ph3.close()
    ph23.close()
    es_hT_done = True

    if stop_after >= 4:
        ph4 = ExitStack()
        hT2 = sb("hT2", [128, 16, 1024], BF16, ph4)
        BhT2 = Buf("hT2")
        G = sb("G", [128, 8, NE + 1], F32, ph4)
        BG = Buf("G")
        wrt = sb("wrt", [128, 16, NE], F32, ph4)
        dma_in(ldc, wrt[:], wr_d, [Bconst])
        Bconst.w = (ldc, S.cnt[ldc])
        Bx1s = Buf("x1_s")
        x1st = S.new_sem("x1st")
        for half in range(2):
            p4 = ExitStack()
            wout = sb("wout", [128, 16, D], BF16, p4)
            Bwout = Buf("wout")
            wosem = S.new_sem(f"wo{half}")
            for q4 in range(4):
                dma_in(wosem, wout[:, q4 * 4:(q4 + 1) * 4, :], wout_d[:, q4 * 4:(q4 + 1) * 4, :], [Bwout], queue="pool")
            mt = [sb(f"mt{i}", [128, 16, 128], BF16, p4) for i in range(2)]
            Bmt = [Buf(f"mt{i}") for i in range(2)]
            mtsem = [S.new_sem(f"mt{half}{i}") for i in range(2)]
            xo = [sb(f"xo{i}", [128, D], F32, p4) for i in range(2)]
            Bxo = [Buf(f"xo{i}") for i in range(2)]
            xosem = [S.new_sem(f"xo{half}{i}") for i in range(2)]
            tmp = sb("tmp4", [128, D], F32, p4)
            Btmp = Buf("tmp4")
            x1t = sb("x1t", [128, D], F32, p4)
            Bx1t = Buf("x1t")
            xn2 = sb("xn2", [128, D], F32, p4)
            Bxn2 = Buf("xn2")
            junk4 = sb("junk4", [128, D], BF16, p4)
            Bjunk4 = Buf("junk4")
            h2f = sb("h2f", [128, 16, 128], F32, p4)
            Bh2f = Buf("h2f")
            st4 = sb("st4", [128, 16], F32, p4)
            Bst4 = Buf("st4")
            rt = {n: sb(f"rt_{n}", shp, F32, p4) for n, shp in
                  (("sc", [128, NE]), ("ch", [128, NE]), ("m8", [128, 8, 8]), ("gs", [128, 8]), ("g8", [128, 8]),
                   ("gm", [128, 8]), ("mch", [128, NE]), ("t8", [128, 8]), ("sel", [128, NE]), ("den", [128, 2]))}
            Brt = Buf("rt")
            for tl in range(8):
                tg_ = half * 8 + tl
                s = tl % 2
                dma_in(mtsem[s], mt[s][:], mrg_s[:, :, tg_ * 128:(tg_ + 1) * 128].rearrange("c p t -> p c t"),
                       [Bmt[s]], reads=[Bmrgs] if stop_after >= 3 else [])
                dma_in(xosem[s], xo[s][:], xe[8 + tg_ * 128:8 + (tg_ + 1) * 128, :], [Bxo[s]])
                for cg in range(4):
                    mm_group(pb[cg][:], [(mt[s][:, kc, :], wout[:, kc, cg * 512:(cg + 1) * 512]) for kc in range(16)],
                             [Bmt[s], Bwout], [BP[cg]])
                for cg in range(4):
                    act(tmp[:, cg * 512:(cg + 1) * 512], pb[cg][:], AF.Copy, [BP[cg]], [Btmp])
                act(junk4[:], tmp[:], AF.Square, [Btmp], [Bjunk4, Bst4], accum_out=st4[:, 4:5])
                rstd_from_ssq(st4[:, 4:5], st4[:, 5:6], Bst4, Bst4, 1.0 / D)
                stt("dve", tmp[:], tmp[:], st4[:, 5:6], gg[:, 0, :], ALU.mult, ALU.mult, [Btmp, Bst4, Bgg], [Btmp])
                tt("pool", x1t[:], tmp[:], xo[s][:], ALU.add, [Btmp, Bxo[s]], [Bx1t])
                dma_in(x1st, x1_s[tg_ * 128:(tg_ + 1) * 128, :], x1t[:], [Bx1s], reads=[Bx1t])
                act(junk4[:], x1t[:], AF.Square, [Bx1t], [Bjunk4, Bst4], accum_out=st4[:, 6:7])
                rstd_from_ssq(st4[:, 6:7], st4[:, 7:8], Bst4, Bst4, 1.0 / D)
                ts("dve", xn2[:], x1t[:], st4[:, 7:8], None, ALU.mult, None, [Bx1t, Bst4], [Bxn2])
                for q4 in range(4):
                    bk = 4 + q4

                    def emit(e, q4=q4, bk=bk):
                        ins = None
                        for j in range(4):
                            c = q4 * 4 + j
                            ins = e.transpose(pb[bk][:, j * 128:(j + 1) * 128], xn2[:, c * 128:(c + 1) * 128], ident[:])
                        return ins
                    S.op("pe", emit, [Bxn2, Bconst], [BP[bk]])
                    for j in range(4):
                        c = q4 * 4 + j
                        act(h2f[:, c, :], pb[bk][:, j * 128:(j + 1) * 128], AF.Identity, [BP[bk], Bmod], [Bh2f],
                            bias=sh_f[:, c:c + 1], scale=A_f[:, c:c + 1])
                cp("pool", hT2[:, :, tl * 128:(tl + 1) * 128], h2f[:], [Bh2f], [BhT2])
                mm_group(pb[0][:, 0:NE], [(h2f[:, c, :], wrt[:, c, :]) for c in range(16)], [Bh2f, Bconst], [BP[0]])
                act(rt["sc"][:], pb[0][:, 0:NE], AF.Sigmoid, [BP[0]], [Brt])
                tt("dve", rt["ch"][:], rt["sc"][:], rbias[:], ALU.add, [Brt, Bconst], [Brt])
                for g8 in range(8):
                    S.op("dve", lambda e, g8=g8: e.max(out=rt["m8"][:, g8, :], in_=rt["ch"][:, g8 * 8:(g8 + 1) * 8]),
                         [Brt], [Brt])
                tt("dve", rt["gs"][:], rt["m8"][:, :, 0], rt["m8"][:, :, 1], ALU.add, [Brt], [Brt])
                S.op("dve", lambda e: e.max(out=rt["g8"][:], in_=rt["gs"][:]), [Brt], [Brt])
                ts("dve", rt["gm"][:], rt["gs"][:], rt["g8"][:, 3:4], None, ALU.is_ge, None, [Brt], [Brt])
                ts("dve", rt["gm"][:], rt["gm"][:], 10.0, -10.0, ALU.mult, ALU.add, [Brt], [Brt])
                tt("dve", rt["mch"][:].rearrange("p (g e) -> p g e", g=8), rt["ch"][:].rearrange("p (g e) -> p g e", g=8),
                   rt["gm"][:].unsqueeze(2).to_broadcast([128, 8, 8]), ALU.add, [Brt], [Brt])
                S.op("dve", lambda e: e.max(out=rt["t8"][:], in_=rt["mch"][:]), [Brt], [Brt])
                ts("dve", rt["sel"][:], rt["mch"][:], rt["t8"][:, 5:6], None, ALU.is_ge, None, [Brt], [Brt])
                tt("dve", rt["sel"][:], rt["sel"][:], rt["sc"][:], ALU.mult, [Brt], [Brt])
                S.op("dve", lambda e: e.reduce_sum(out=rt["den"][:, 0:1], in_=rt["sel"][:], axis=AX.X), [Brt], [Brt])
                S.op("dve", lambda e: e.reciprocal(out=rt["den"][:, 1:2], in_=rt["den"][:, 0:1]), [Brt], [Brt])
                ts("dve", G[:, tl, 0:NE], rt["sel"][:], rt["den"][:, 1:2], 2.5, ALU.mult, ALU.mult, [Brt], [BG])
                S.op("dve", lambda e, tl=tl: e.memset(G[:, tl, NE:NE + 1], 1.0), [], [BG])
            if dbg:
                dma_in(st_sem, dbg_out["d_hT2"][:, :, half * 1024:(half + 1) * 1024], hT2[:], [], reads=[BhT2])
                dma_in(st_sem, dbg_out["d_G"][:, half * 8:(half + 1) * 8, :], G[:, :, 0:NE], [], reads=[BG])
            S.barrier()
            p4.close()
            if stop_after < 5:
                continue
            p5 = ExitStack()
            acc = sb("acc", [128, 8, D], F32, p5)
            Bacc = [Buf(f"acc{i}") for i in range(8)]
            p5a = ExitStack()
            NGU = 4
            wgr = [sb(f"wgr{i}", [128, 16, 128], BF16, p5a) for i in range(NGU)]
            wur = [sb(f"wur{i}", [128, 16, 128], BF16, p5a) for i in range(NGU)]
            Bgu = [Buf(f"gu{i}") for i in range(NGU)]
            gusem = [S.new_sem(f"gu{half}{i}") for i in range(NGU)]
            NDW = 3
            wdr = [sb(f"wdr{i}", [128, NF, 256], BF16, p5a) for i in range(NDW)]
            Bwd = [Buf(f"wd{i}") for i in range(NDW)]
            wdsem = [S.new_sem(f"wd{half}{i}") for i in range(NDW)]
            actT1 = sb("actT", [128, NF, 1024], BF16, p5a)
            actT = [actT1, actT1]
            BactT1 = Buf("actT")
            BactT = [BactT1, BactT1]
            slt = [sb(f"slt{i}", [128, 512], F32, p5a) for i in range(2)]
            Bslt = [Buf(f"slt{i}") for i in range(2)]
            gui = 0
            wdi = 0
            pbi = 0
            sli = 0
            for e_ in range(NE + 1):
                a_i = e_ % 2
                for f in range(NF):
                    r = gui % NGU
                    gui += 1
                    dma_in(gusem[r], wgr[r][:], weg_d[e_, f], [Bgu[r]], queue="pool")
                    dma_in(gusem[r], wur[r][:], weu_d[e_, f], [Bgu[r]], queue="pool")
                    for tg_ in range(2):
                        gb = (pbi % 3) * 2
                        pbi += 1
                        ub = gb + 1
                        tk = slice(tg_ * 512, (tg_ + 1) * 512)
                        mm_group(pb[gb][:], [(wgr[r][:, kc, :], hT2[:, kc, tk]) for kc in range(16)],
                                 [Bgu[r], BhT2], [BP[gb]])
                        mm_group(pb[ub][:], [(wur[r][:, kc, :], hT2[:, kc, tk]) for kc in range(16)],
                                 [Bgu[r], BhT2], [BP[ub]])
                        si = sli % 2
                        sli += 1
                        act(slt[si][:], pb[gb][:], AF.Silu, [BP[gb]], [Bslt[si]])
                        tt("dve", actT[a_i][:, f, tk], slt[si][:], pb[ub][:], ALU.mult, [Bslt[si], BP[ub]], [BactT[a_i]])
                for c8 in range(8):
                    r = wdi % NDW
                    wdi += 1
                    dma_in(wdsem[r], wdr[r][:], wed_d[e_, c8], [Bwd[r]], queue="pool")
                    for tl in range(8):
                        bk = 6 + (tl % 2)
                        mm_group(pb[bk][:, 0:256], [(actT[a_i][:, f, tl * 128:(tl + 1) * 128], wdr[r][:, f, :])
                                                    for f in range(NF)], [BactT[a_i], Bwd[r]], [BP[bk]])
                        dst = acc[:, tl, c8 * 256:(c8 + 1) * 256]
                        if e_ == 0:
                            ts("dve", dst, pb[bk][:, 0:256], G[:, tl, e_:e_ + 1], None, ALU.mult, None,
                               [BP[bk], BG], [Bacc[tl]])
                        else:
                            stt("dve", dst, pb[bk][:, 0:256], G[:, tl, e_:e_ + 1], dst, ALU.mult, ALU.add,
                                [BP[bk], BG, Bacc[tl]], [Bacc[tl]])
            S.barrier()
            p5a.close()
            x1r = [sb(f"x1r{i}", [128, D], F32, p5) for i in range(2)]
            Bx1r = [Buf(f"x1r{i}") for i in range(2)]
            x1sem = [S.new_sem(f"x1r{half}{i}") for i in range(2)]
            fo = [sb(f"fo{i}", [128, D], F32, p5) for i in range(2)]
            Bfo = [Buf(f"fo{i}") for i in range(2)]
            junk5 = sb("junk5", [128, D], BF16, p5)
            Bjunk5 = Buf("junk5")
            st5 = sb("st5", [128, 16], F32, p5)
            Bst5 = Buf("st5")
            for tl in range(8):
                tg_ = half * 8 + tl
                s = tl % 2
                dma_in(x1sem[s], x1r[s][:], x1_s[tg_ * 128:(tg_ + 1) * 128, :], [Bx1r[s]], reads=[Bx1s])
                act(junk5[:], acc[:, tl, :], AF.Square, [Bacc[tl]], [Bjunk5, Bst5], accum_out=st5[:, 2 * s:2 * s + 1])
                rstd_from_ssq(st5[:, 2 * s:2 * s + 1], st5[:, 2 * s + 1:2 * s + 2], Bst5, Bst5, 1.0 / D)
                stt("dve", fo[s][:], acc[:, tl, :], st5[:, 2 * s + 1:2 * s + 2], gg[:, 1, :], ALU.mult, ALU.mult,
                    [Bacc[tl], Bst5, Bgg], [Bfo[s]])
                tt("pool", fo[s][:], fo[s][:], x1r[s][:], ALU.add, [Bfo[s], Bx1r[s]], [Bfo[s]])
                dma_in(st_sem, out_d[tg_ * 128:(tg_ + 1) * 128, :], fo[s][:], [], reads=[Bfo[s]])
            S.barrier()
            p5.close()
        if dbg:
            dma_in(st_sem, dbg_out["d_x1"], x1_s, [], reads=[Bx1s])
        ph4.close()

    S.barrier()

    block = ec(nc.Block())

    @block.sync
    def _(e):
        S.replay("sp", e)

    @block.scalar
    def _(e):
        S.replay("act", e)

    @block.vector
    def _(e):
        S.replay("dve", e)

    @block.gpsimd
    def _(e):
        S.replay("pool", e)

    @block.tensor
    def _(e):
        S.replay("pe", e)

    es.close()
    return nc


def _rope_tables():
    rows = S_LEN // 64
    t = np.arange(S_LEN)
    row = (t // 64).astype(np.float32)
    col = (t % 64).astype(np.float32)
    inv_freq = (np.float32(10000.0) ** (-np.arange(0, 64, 2, dtype=np.float32) / np.float32(64))).astype(np.float32)
    cosT = np.zeros((128, S_LEN), np.float32)
    sinT = np.zeros((128, S_LEN), np.float32)
    perm = np.zeros((128, 128), np.float32)
    for d in range(128):
        axis, r = d // 64, d % 64
        half, i = r // 32, r % 32
        pos = row if axis == 0 else col
        ang = (pos * inv_freq[i]).astype(np.float32)
        cosT[d] = np.cos(ang)
        sinT[d] = -np.sin(ang) if half == 0 else np.sin(ang)
        partner = d + 32 if half == 0 else d - 32
        perm[partner, d] = 1.0
    return cosT, sinT, perm


def _ktile(w, nk=16):
    K, M = w.shape
    return np.ascontiguousarray(w.reshape(K // 128, 128, M).transpose(1, 0, 2))


def _prep_shared(inp, with_experts=True):
    f = np.float32
    w_in = np.asarray(inp["w_in"], f)[0]
    sh = {}
    sh["wmod_l"] = np.ascontiguousarray(np.asarray(inp["w_mod"], f)[0].reshape(16, 128, 24, 512).transpose(2, 1, 0, 3))
    b_mod = np.asarray(inp["b_mod"], f)[0]
    sh["bmodT"] = np.ascontiguousarray(b_mod.reshape(96, 128).T)
    sh["bgt_rep"] = np.ascontiguousarray(np.broadcast_to(
        np.stack([b_mod[4096:6144], b_mod[10240:12288]])[None], (128, 2, D)))
    sh["gpost_rep"] = np.ascontiguousarray(np.broadcast_to(
        np.stack([np.asarray(inp["g_post_mix"], f)[0], np.asarray(inp["g_post_ffn"], f)[0]])[None], (128, 2, D)))
    sh["gpreT"] = np.ascontiguousarray(np.stack([np.asarray(inp["g_pre_mix"], f)[0].reshape(16, 128).T,
                                                 np.asarray(inp["g_pre_ffn"], f)[0].reshape(16, 128).T], axis=1))
    sh["qkg"] = np.ascontiguousarray(np.stack([np.asarray(inp["q_norm_g"], f)[0], np.asarray(inp["k_norm_g"], f)[0]], axis=1))
    sh["wk_l"] = _ktile(w_in[:, 2048:2560])
    sh["wv_l"] = _ktile(w_in[:, 2560:3072])

    def heads(wc):
        return np.ascontiguousarray(wc.reshape(16, 128, 16, 128).transpose(2, 1, 0, 3))
    sh["wq_l"] = heads(w_in[:, 0:2048])
    sh["wp_l"] = heads(w_in[:, 3072:5120])
    sh["wga_l"] = heads(w_in[:, 5120:7168])
    sh["wgp_l"] = heads(w_in[:, 7168:9216])
    sh["wpool_l"] = np.ascontiguousarray(np.asarray(inp["w_pool"], f)[0].reshape(4, 4, 128, 512).transpose(0, 2, 1, 3))
    sh["pscT"] = np.ascontiguousarray(np.asarray(inp["pool_scale"], f)[0].reshape(16, 128).T)
    sh["wout_l"] = _ktile(np.asarray(inp["w_out"], f)[0])
    sh["wr_l"] = _ktile(np.asarray(inp["w_router"], f)[0])
    sh["rbias_rep"] = np.ascontiguousarray(np.broadcast_to(np.asarray(inp["router_bias"], f)[0][None], (128, NE)))

    sh["ident"] = np.eye(128, dtype=f)
    cosT, sinT, perm = _rope_tables()
    sh["perm"] = perm
    sh["cosk"] = cosT
    sh["sink"] = sinT

    def gu(we, ws):
        w = np.concatenate([np.asarray(we, f)[0], np.asarray(ws, f)[0][None]], axis=0)
        return np.ascontiguousarray(w.reshape(NE + 1, 16, 128, NF, 128).transpose(0, 3, 2, 1, 4))
    if not with_experts:
        return sh, cosT, sinT
    sh["weg_l"] = gu(inp["w_exp_gate"], inp["w_sh_gate"])
    sh["weu_l"] = gu(inp["w_exp_up"], inp["w_sh_up"])
    wd = np.concatenate([np.asarray(inp["w_exp_down"], f)[0], np.asarray(inp["w_sh_down"], f)[0][None]], axis=0)
    sh["wed_l"] = np.ascontiguousarray(wd.reshape(NE + 1, NF, 128, 8, 256).transpose(0, 3, 2, 1, 4))
    return sh, cosT, sinT


def _prep_core(inp, sh, cosT, sinT, core):
    f = np.float32
    x = np.asarray(inp["x"], f)
    c = np.asarray(inp["c"], f)
    b, pos = core // 4, core % 4
    t0 = pos * NOWN
    m = dict(sh)
    m["xb"] = x[b]
    xe = np.zeros((NEXT, D), f)
    xe[8:8 + NOWN] = x[b, t0:t0 + NOWN]
    if pos > 0:
        xe[0:8] = x[b, t0 - 8:t0]
    if pos < 3:
        xe[8 + NOWN:] = x[b, t0 + NOWN:t0 + NOWN + 8]
    m["xe"] = xe
    m["cT"] = np.ascontiguousarray(c[b].reshape(16, 128).T)
    m["cosq"] = np.ascontiguousarray(cosT[:, t0:t0 + NOWN])
    m["sinq"] = np.ascontiguousarray(sinT[:, t0:t0 + NOWN])
    m["hflag"] = np.ascontiguousarray(np.broadcast_to(np.array([pos > 0, pos < 3], f)[None], (128, 2)))
    ie = np.zeros((4, 16), f)
    for wi, w in enumerate((2, 4, 8, 16)):
        for j in range(16):
            t = t0 + j if j < 8 else t0 + NOWN - 8 + (j - 8)
            lo = max(t - w // 2, 0)
            hi = min(t - w // 2 + w, S_LEN)
            ie[wi, j] = 1.0 / float(hi - lo)
    m["iedge"] = np.ascontiguousarray(np.broadcast_to(ie[None], (128, 4, 16)))
    return m


def kernel(**inputs):
    import time
    _t0 = time.time()
    stop_after = DEBUG.get("stop_after", 99)
    sh, cosT, sinT = _prep_shared(inputs, with_experts=stop_after >= 5)
    in_maps = [_prep_core(inputs, sh, cosT, sinT, core) for core in range(8)]
    dbg = DEBUG.get("dbg", False)
    _t1 = time.time()
    nc = build_program(stop_after=stop_after, dbg=dbg)
    _t2 = time.time()
    ncores = DEBUG.get("ncores", 8)
    if DEBUG.get("trace"):
        res = run_bass_kernel_spmd(nc, in_maps[:ncores], core_ids=list(range(ncores)), trace=True)
        print("[kernel] exec_time_ns", res.exec_time_ns, flush=True)
    else:
        res = run_bass_kernel_spmd(nc, in_maps[:ncores], core_ids=list(range(ncores)))
    print(f"[kernel] prep {_t1 - _t0:.1f}s build {_t2 - _t1:.1f}s run {time.time() - _t2:.1f}s", flush=True)
    DEBUG["res"] = res
    out = np.zeros((2, S_LEN, D), np.float32)
    for core in range(ncores):
        b, pos = core // 4, core % 4
        out[b, pos * NOWN:(pos + 1) * NOWN] = np.asarray(res.results[core]["out"], np.float32)
    return out
```

```python
import numpy as np
from contextlib import ExitStack
import concourse.bass as bass
import concourse.mybir as mybir
from concourse.bass_utils import run_bass_kernel_spmd

F32 = mybir.dt.float32
BF16 = mybir.dt.bfloat16
AF = mybir.ActivationFunctionType
ALU = mybir.AluOpType
AX = mybir.AxisListType

D = 2048
S_LEN = 8192
NOWN = 2048
NEXT = NOWN + 16
FF = 1408
NF = 11
NE = 64
EPS = 1e-6
ENG_NAMES = ("pe", "act", "dve", "pool", "sp")

DEBUG = {}


class Buf:
    __slots__ = ("name", "w", "r")

    def __init__(self, name):
        self.name = name
        self.w = None
        self.r = []


class Sched:
    def __init__(self, nc, ec):
        self.nc = nc
        self.ec = ec
        self.streams = {e: [] for e in ENG_NAMES}
        self.cnt = {}
        self.sems = {}
        self.waited = {e: {} for e in ENG_NAMES}
        for e in ("pe", "act", "dve", "pool"):
            self.new_sem(e)

    def new_sem(self, key):
        self.sems[key] = self.ec(self.nc.semaphore(key))
        self.cnt[key] = 0
        return key

    def _waits(self, eng, deps):
        need = {}
        for tok in deps:
            if tok is None:
                continue
            k, v = tok
            if k == "pe" and eng == "pe":
                continue
            if v > need.get(k, 0):
                need[k] = v
        out = []
        wd = self.waited[eng]
        for k, v in need.items():
            if wd.get(k, 0) >= v:
                continue
            wd[k] = v
            out.append((k, v))
        return out

    @staticmethod
    def _deps(reads, writes):
        deps = []
        for b in reads:
            if b.w is not None:
                deps.append(b.w)
        for b in writes:
            if b.w is not None:
                deps.append(b.w)
            deps.extend(b.r)
        return deps

    @staticmethod
    def _commit(tok, reads, writes):
        for b in reads:
            b.r.append(tok)
            if len(b.r) > 64:
                best = {}
                for k, v in b.r:
                    if v > best.get(k, 0):
                        best[k] = v
                b.r = list(best.items())
        for b in writes:
            b.w = tok
            b.r = []

    def op(self, eng, emit, reads=(), writes=()):
        waits = self._waits(eng, self._deps(reads, writes))
        self.cnt[eng] += 1
        tok = (eng, self.cnt[eng])
        self.streams[eng].append((waits, emit, (eng, 1)))
        self._commit(tok, reads, writes)
        return tok

    def dma(self, queue, semkey, emit, reads=(), writes=()):
        waits = self._waits(queue, self._deps(reads, writes))
        self.cnt[semkey] += 16
        tok = (semkey, self.cnt[semkey])
        self.streams[queue].append((waits, emit, (semkey, 16)))
        self._commit(tok, reads, writes)
        return tok

    def barrier(self):
        allv = [(k, v) for k, v in self.cnt.items() if v > 0]
        for e in ENG_NAMES:
            waits = self._waits(e, allv)
            if waits:
                self.streams[e].append((waits, None, None))

    def replay(self, eng, eobj):
        sems = self.sems
        for waits, emit, inc in self.streams[eng]:
            for k, v in waits:
                eobj.wait_ge(sems[k], v)
            if emit is not None:
                ins = emit(eobj)
                ins.then_inc(sems[inc[0]], inc[1])


def build_program(stop_after=99, dbg=False):
    nc = bass.Bass("TRN2", target_bir_lowering=False)
    es = ExitStack()
    ec = es.enter_context

    def din(name, shape, dt=F32):
        return nc.dram_tensor(name, list(shape), dt, kind="ExternalInput").ap()

    xb = din("xb", [S_LEN, D])
    xe = din("xe", [NEXT, D])
    cT_d = din("cT", [128, 16])
    wmod_d = din("wmod_l", [24, 128, 16, 512])
    bmodT_d = din("bmodT", [128, 96])
    bgt_d = din("bgt_rep", [128, 2, D])
    gpost_d = din("gpost_rep", [128, 2, D])
    gpre_d = din("gpreT", [128, 2, 16])
    qkg_d = din("qkg", [128, 2])
    wk_d = din("wk_l", [128, 16, 512])
    wv_d = din("wv_l", [128, 16, 512])
    wq_d = din("wq_l", [16, 128, 16, 128])
    wp_d = din("wp_l", [16, 128, 16, 128])
    wga_d = din("wga_l", [16, 128, 16, 128])
    wgp_d = din("wgp_l", [16, 128, 16, 128])
    wpool_d = din("wpool_l", [4, 128, 4, 512])
    pscT_d = din("pscT", [128, 16])
    wout_d = din("wout_l", [128, 16, D])
    wr_d = din("wr_l", [128, 16, NE])
    rbias_d = din("rbias_rep", [128, NE])
    if stop_after >= 5:
        weg_d = din("weg_l", [NE + 1, NF, 128, 16, 128])
        weu_d = din("weu_l", [NE + 1, NF, 128, 16, 128])
        wed_d = din("wed_l", [NE + 1, 8, 128, NF, 256])
    ident_d = din("ident", [128, 128])
    perm_d = din("perm", [128, 128])
    cosk_d = din("cosk", [128, S_LEN])
    sink_d = din("sink", [128, S_LEN])
    cosq_d = din("cosq", [128, NOWN])
    sinq_d = din("sinq", [128, NOWN])
    hflag_d = din("hflag", [128, 2])
    iedge_d = din("iedge", [128, 4, 16])
    out_d = nc.dram_tensor("out", [NOWN, D], F32, kind="ExternalOutput").ap()
    kT_s = nc.dram_tensor("kT_s", [4, 128, S_LEN], BF16).ap()
    v_s = nc.dram_tensor("v_s", [4, 128, 64 * 128], BF16).ap()
    mrg_s = nc.dram_tensor("mrg_s", [16, 128, NOWN], BF16).ap()
    x1_s = nc.dram_tensor("x1_s", [NOWN, D], F32).ap()
    dbg_out = {}
    if dbg:
        dbg_out["d_hT"] = nc.dram_tensor("d_hT", [128, 16, NEXT], BF16, kind="ExternalOutput").ap()
        dbg_out["d_G"] = nc.dram_tensor("d_G", [128, 16, NE], F32, kind="ExternalOutput").ap()
        dbg_out["d_hT2"] = nc.dram_tensor("d_hT2", [128, 16, NOWN], BF16, kind="ExternalOutput").ap()
        dbg_out["d_kT"] = nc.dram_tensor("d_kT", [4, 128, S_LEN], BF16, kind="ExternalOutput").ap()
        dbg_out["d_v"] = nc.dram_tensor("d_v", [4, 128, 64 * 128], BF16, kind="ExternalOutput").ap()
        dbg_out["d_mrg"] = nc.dram_tensor("d_mrg", [16, 128, NOWN], BF16, kind="ExternalOutput").ap()
        dbg_out["d_x1"] = nc.dram_tensor("d_x1", [NOWN, D], F32, kind="ExternalOutput").ap()

    S = Sched(nc, ec)
    block = None

    sbc = [0]

    def sb(name, shape, dt, stack=None):
        sbc[0] += 1
        return (stack or es).enter_context(nc.sbuf_tensor(f"{name}_{sbc[0]}", list(shape), dt))

    pb = [ec(nc.psum_tensor(f"pb{i}", [128, 512], F32)) for i in range(8)]
    BP = [Buf(f"pb{i}") for i in range(8)]

    ident = sb("ident", [128, 128], F32)
    identb = sb("identb", [128, 128], BF16)
    permb = sb("permb", [128, 128], BF16)
    onesb = sb("onesb", [128, 128], BF16)
    ones32 = sb("ones32", [128, 128], F32)
    epsc = sb("epsc", [128, 1], F32)
    cT = sb("cT", [128, 16], F32)
    modT = sb("modT", [128, 96], F32)
    gpre = sb("gpre", [128, 2, 16], F32)
    A_a = sb("A_a", [128, 16], F32)
    A_f = sb("A_f", [128, 16], F32)
    qkg = sb("qkg", [128, 2], F32)
    pscT = sb("pscT", [128, 16], F32)
    hflag = sb("hflag", [128, 2], F32)
    iedge = sb("iedge", [128, 4, 16], F32)
    gg = sb("gg", [128, 2, D], F32)
    rbias = sb("rbias", [128, NE], F32)
    Bconst = Buf("const")
    Bmod = Buf("mod")
    Bgg = Buf("gg")

    ldc = S.new_sem("ldc")
    st_sem = S.new_sem("st")

    def dma_in(sem, out_ap, in_ap, writes, reads=(), queue="sp"):
        return S.dma(queue, sem, lambda e: e.dma_start(out=out_ap, in_=in_ap), reads=reads, writes=writes)

    def mm_group(out_ap, pairs, reads, writes):
        def emit(e):
            n = len(pairs)
            ins = None
            for i, (l, r) in enumerate(pairs):
                ins = e.matmul(out_ap, lhsT=l, rhs=r, start=(i == 0), stop=(i == n - 1))
            return ins
        return S.op("pe", emit, reads, writes)

    def act(out, in_, func, reads, writes, **kw):
        return S.op("act", lambda e: e.activation(out=out, in_=in_, func=func, **kw), reads, writes)

    def ts(eng, out, in0, s1, s2, op0, op1, reads, writes):
        if op1 is None:
            return S.op(eng, lambda e: e.tensor_scalar(out=out, in0=in0, scalar1=s1, scalar2=None, op0=op0), reads, writes)
        return S.op(eng, lambda e: e.tensor_scalar(out=out, in0=in0, scalar1=s1, scalar2=s2, op0=op0, op1=op1), reads, writes)

    def tt(eng, out, in0, in1, op, reads, writes):
        return S.op(eng, lambda e: e.tensor_tensor(out=out, in0=in0, in1=in1, op=op), reads, writes)

    def stt(eng, out, in0, scalar, in1, op0, op1, reads, writes):
        return S.op(eng, lambda e: e.scalar_tensor_tensor(out=out, in0=in0, scalar=scalar, in1=in1, op0=op0, op1=op1),
                    reads, writes)

    def cp(eng, out, in_, reads, writes):
        return S.op(eng, lambda e: e.tensor_copy(out=out, in_=in_), reads, writes)

    def rstd_from_ssq(ssq_ap, out_ap, Bssq, Bout, inv_n):
        act(out_ap, ssq_ap, AF.Sqrt, [Bssq, Bconst], [Bout], bias=epsc[:, 0:1], scale=inv_n)
        S.op("dve", lambda e: e.reciprocal(out=out_ap, in_=out_ap), [Bout], [Bout])

    ph0 = ExitStack()
    for (t, d_) in ((ident, ident_d), (cT, cT_d), (modT, bmodT_d), (gpre, gpre_d), (qkg, qkg_d), (pscT, pscT_d),
                    (hflag, hflag_d), (iedge, iedge_d), (rbias, rbias_d)):
        dma_in(ldc, t[:], d_, [Bconst])
    perm32 = sb("perm32", [128, 128], F32, ph0)
    dma_in(ldc, perm32[:], perm_d, [Bconst])
    bgt = sb("bgt", [128, 2, D], F32, ph0)
    gpo = sb("gpo", [128, 2, D], F32, ph0)
    dma_in(ldc, bgt[:], bgt_d, [Bconst])
    dma_in(ldc, gpo[:], gpost_d, [Bconst])
    Bconst.w = (ldc, S.cnt[ldc])
    S.op("dve", lambda e: e.memset(onesb[:], 1.0), [], [Bconst])
    S.op("dve", lambda e: e.memset(ones32[:], 1.0), [], [Bconst])
    S.op("dve", lambda e: e.memset(epsc[:], EPS), [], [Bconst])
    cp("dve", identb[:], ident[:], [Bconst], [Bconst])
    cp("dve", permb[:], perm32[:], [Bconst], [Bconst])
    act(cT[:], cT[:], AF.Silu, [Bconst], [Bconst])
    cTb = sb("cTb", [128, 16, 128], F32, ph0)
    for kc in range(16):
        act(cTb[:, kc, :], ones32[:], AF.Copy, [Bconst], [Bconst], scale=cT[:, kc:kc + 1])
    wm = [sb(f"wm{i}", [128, 16, 512], F32, ph0) for i in range(2)]
    Bwm = [Buf(f"wm{i}") for i in range(2)]
    wm_sem = [S.new_sem(f"wm{i}") for i in range(2)]
    Bpm = BP[0]
    first_fm = True
    fm_blocks = list(range(0, 8)) + list(range(12, 20))
    for blk in range(24):
        s = blk % 2
        dma_in(wm_sem[s], wm[s][:], wmod_d[blk], [Bwm[s]])
        if blk in fm_blocks:
            for jj in range(4):
                j = blk * 4 + jj
                pairs = [(wm[s][:, kc, jj * 128:(jj + 1) * 128], cT[:, kc:kc + 1]) for kc in range(16)]
                mm_group(pb[0][:, j:j + 1], pairs, [Bwm[s], Bconst], [Bpm])
        else:
            gi = 0 if blk < 12 else 1
            cb = (blk - 8) if blk < 12 else (blk - 20)
            pairs = [(cTb[:, kc, :], wm[s][:, kc, :]) for kc in range(16)]
            bank = 1 + (blk % 2)
            mm_group(pb[bank][:], pairs, [Bwm[s], Bconst], [BP[bank]])
            sl = slice(cb * 512, (cb + 1) * 512)
            tt("dve", gg[:, gi, sl], pb[bank][:], bgt[:, gi, sl], ALU.add, [BP[bank], Bconst], [Bgg])
            tt("dve", gg[:, gi, sl], gg[:, gi, sl], gpo[:, gi, sl], ALU.mult, [Bgg, Bconst], [Bgg])
    for (a, b_) in ((0, 32), (48, 80)):
        tt("dve", modT[:, a:b_], pb[0][:, a:b_], modT[:, a:b_], ALU.add, [Bpm, Bconst], [Bmod])
    stt("dve", A_a[:], modT[:, 16:32], 1.0, gpre[:, 0, :], ALU.add, ALU.mult, [Bmod, Bconst], [Bmod])
    stt("dve", A_f[:], modT[:, 64:80], 1.0, gpre[:, 1, :], ALU.add, ALU.mult, [Bmod, Bconst], [Bmod])
    sh_a = modT[:, 0:16]
    sh_f = modT[:, 48:64]
    S.barrier()
    ph0.close()

    def norm_transpose_group(stack_bufs, x_src_ap, dst_fn, Bdst, Aap, shap, tbanks, prefetch=None):
        (xt, Bxt, xsem, ssq, Bssq, xn, Bxn, junk, Bjunk) = stack_bufs
        if x_src_ap is not None:
            dma_in(xsem, xt[:], x_src_ap, [Bxt])
        for t in range(4):
            act(junk[:], xt[:, t, :], AF.Square, [Bxt], [Bjunk, Bssq], accum_out=ssq[:, t:t + 1])
        rstd_from_ssq(ssq[:, 0:4], ssq[:, 4:8], Bssq, Bssq, 1.0 / D)
        for t in range(4):
            if t % 2 == 0:
                ts("dve", xn[:, t, :], xt[:, t, :], ssq[:, 4 + t:5 + t], None, ALU.mult, None, [Bxt, Bssq], [Bxn])
            else:
                act(xn[:, t, :], xt[:, t, :], AF.Copy, [Bxt, Bssq], [Bxn], scale=ssq[:, 4 + t:5 + t])
        if prefetch is not None:
            prefetch()
        for c in range(16):
            bk = tbanks[c % 2]

            def emit(e, c=c, bk=bk):
                ins = None
                for t in range(4):
                    ins = e.transpose(pb[bk][:, t * 128:(t + 1) * 128], xn[:, t, c * 128:(c + 1) * 128], ident[:])
                return ins
            S.op("pe", emit, [Bxn, Bconst], [BP[bk]])
            act(dst_fn(c), pb[bk][:], AF.Identity, [BP[bk], Bmod], [Bdst], bias=shap[:, c:c + 1], scale=Aap[:, c:c + 1])

    def qk_post(psrc_bank, gcol, cos_ap, sin_ap, Btab, out_ap, Bout, tmp, Btmp, abank, rbank):
        (sq, tg, u, w, rs) = tmp
        ps = pb[psrc_bank]
        lim = DEBUG.get("qkstage", 99)
        if lim <= 0:
            return
        act(sq[:], ps[:], AF.Copy, [BP[psrc_bank]], [Btmp[0]])
        stt("dve", sq[:], ps[:], 1.0, sq[:], ALU.mult, ALU.mult, [BP[psrc_bank], Btmp[0]], [Btmp[0]])
        if lim <= 1:
            return
        ts("dve", tg[:], ps[:], qkg[:, gcol:gcol + 1], None, ALU.mult, None, [BP[psrc_bank], Bconst], [Btmp[1]])
        if lim <= 2:
            return
        mm_group(pb[abank][:], [(onesb[:], sq[:])], [Btmp[0], Bconst], [BP[abank]])
        if lim <= 3:
            return
        mm_group(pb[rbank][:], [(permb[:], tg[:])], [Btmp[1], Bconst], [BP[rbank]])
        if lim <= 4:
            return
        act(rs[:], pb[abank][:], AF.Sqrt, [BP[abank], Bconst], [Btmp[4]], bias=epsc[:, 0:1], scale=1.0 / 128)
        if lim <= 5:
            return
        S.op("dve", lambda e: e.reciprocal(out=rs[:], in_=rs[:]), [Btmp[4]], [Btmp[4]])
        if lim <= 6:
            return
        tt(DEBUG.get("qkeng", "pool"), u[:], tg[:], cos_ap, ALU.mult, [Btmp[1], Btab], [Btmp[2]])
        if lim <= 7:
            return
        tt("dve", w[:], pb[rbank][:], sin_ap, ALU.mult, [BP[rbank], Btab], [Btmp[3]])
        if lim <= 8:
            return
        tt(DEBUG.get("qkeng", "pool"), u[:], u[:], w[:], ALU.add, [Btmp[2], Btmp[3]], [Btmp[2]])
        if lim <= 9:
            return
        tt("dve", out_ap, u[:], rs[:], ALU.mult, [Btmp[2], Btmp[4]], [Bout])

    ph1 = ExitStack()
    xt1 = sb("xt", [128, 4, D], F32, ph1)
    xt = [xt1, xt1]
    Bxt1 = Buf("xt")
    Bxt = [Bxt1, Bxt1]
    xs1 = S.new_sem("xs")
    xsem = [xs1, xs1]
    ssq = [sb(f"ssq{i}", [128, 8], F32, ph1) for i in range(2)]
    Bssq = [Buf(f"ssq{i}") for i in range(2)]
    xn = sb("xn", [128, 4, D], F32, ph1)
    Bxn = Buf("xn")
    junk = sb("junk", [128, D], BF16, ph1)
    Bjunk = Buf("junk")
    hTg = sb("hTg", [128, 16, 512], BF16, ph1)
    BhTg = Buf("hTg")
    wk = sb("wk", [128, 16, 512], BF16, ph1)
    wv = sb("wv", [128, 16, 512], BF16, ph1)
    Bwkv = Buf("wkv")
    wsem = S.new_sem("wkv")
    dma_in(wsem, wk[:], wk_d, [Bwkv], queue="pool")
    dma_in(wsem, wv[:], wv_d, [Bwkv], queue="pool")
    ctab = [sb(f"ctab{i}", [128, 2, 512], F32, ph1) for i in range(2)]
    Bctab = [Buf(f"ctab{i}") for i in range(2)]
    csem = [S.new_sem(f"cs{i}") for i in range(2)]
    qtmp = (sb("q_sq", [128, 512], BF16, ph1), sb("q_tg", [128, 512], BF16, ph1), sb("q_u", [128, 512], F32, ph1),
            sb("q_w", [128, 512], F32, ph1), sb("q_rs", [128, 512], F32, ph1))
    Bqtmp = [Buf(f"qtmp{i}") for i in range(5)]
    kTg = [sb(f"kTg{i}", [128, 4, 512], BF16, ph1) for i in range(2)]
    BkTg = [Buf(f"kTg{i}") for i in range(2)]
    vg = [sb(f"vg{i}", [128, 4, 4, 128], BF16, ph1) for i in range(2)]
    Bvg = [Buf(f"vg{i}") for i in range(2)]
    kv_st = S.new_sem("kvst")
    Bks = Buf("kT_s")
    Bvs = Buf("v_s")

    n_kvg = DEBUG.get('kv_groups', 16) if stop_after >= 1 else 0

    def kv_loads(g):
        s_ = g % 2
        dma_in(csem[s_], ctab[s_][:, 0, :], cosk_d[:, g * 512:(g + 1) * 512], [Bctab[s_]])
        dma_in(csem[s_], ctab[s_][:, 1, :], sink_d[:, g * 512:(g + 1) * 512], [Bctab[s_]])
        dma_in(xsem[s_], xt[s_][:], xb[g * 512:(g + 1) * 512, :].rearrange("(t p) d -> p t d", p=128), [Bxt[s_]])
    if n_kvg > 0:
        kv_loads(0)
    for g in range(n_kvg):
        s = g % 2
        norm_transpose_group((xt[s], Bxt[s], xsem[s], ssq[s], Bssq[s], xn, Bxn, junk, Bjunk),
                             None, lambda c: hTg[:, c, :], BhTg, A_a, sh_a, (0, 1),
                             prefetch=(lambda g=g: kv_loads(g + 1)) if g + 1 < n_kvg else None)

        def k_mm(hd):
            bk = 2 + (hd % 2)
            mm_group(pb[bk][:], [(wk[:, kc, hd * 128:(hd + 1) * 128], hTg[:, kc, :]) for kc in range(16)],
                     [Bwkv, BhTg], [BP[bk]])

        def k_post(hd):
            qk_post(2 + (hd % 2), 1, ctab[s][:, 0, :], ctab[s][:, 1, :], Bctab[s], kTg[s][:, hd, :], BkTg[s],
                    qtmp, Bqtmp, 6, 7)

        def v_mm(t):
            bk = 4 + (t % 2)
            mm_group(pb[bk][:], [(hTg[:, kc, t * 128:(t + 1) * 128], wv[:, kc, :]) for kc in range(16)],
                     [Bwkv, BhTg], [BP[bk]])
            act(vg[s][:, :, t, :], pb[bk][:].rearrange("p (h d) -> p h d", h=4), AF.Copy, [BP[bk]], [Bvg[s]])
        for half in range(2):
            k_mm(2 * half)
            k_mm(2 * half + 1)
            v_mm(2 * half)
            v_mm(2 * half + 1)
            k_post(2 * half)
            k_post(2 * half + 1)
        for hd in range(0 if DEBUG.get("nostore") else 4):
            dma_in(kv_st, kT_s[hd, :, g * 512:(g + 1) * 512], kTg[s][:, hd, :], [Bks], reads=[BkTg[s]])
            dma_in(kv_st, v_s[hd, :, g * 512:(g + 1) * 512], vg[s][:, hd, :, :].rearrange("p t d -> p (t d)"),
                   [Bvs], reads=[Bvg[s]])

    if dbg:
        dma_in(st_sem, dbg_out["d_kT"], kT_s, [], reads=[Bks])
        dma_in(st_sem, dbg_out["d_v"], v_s, [], reads=[Bvs])
    S.barrier()
    ph1.close()
    ph23 = ExitStack()
    hT = sb("hT", [128, 16, NEXT], BF16, ph23)
    BhT = Buf("hT")
    ph1 = ExitStack()
    xt1 = sb("xt", [128, 4, D], F32, ph1)
    xt = [xt1, xt1]
    Bxt1 = Buf("xt")
    Bxt = [Bxt1, Bxt1]
    ssq = [sb(f"ssq{i}", [128, 8], F32, ph1) for i in range(2)]
    Bssq = [Buf(f"ssq{i}") for i in range(2)]
    xn = sb("xn", [128, 4, D], F32, ph1)
    Bxn = Buf("xn")
    junk = sb("junk", [128, D], BF16, ph1)
    Bjunk = Buf("junk")
    for g in range(4 if stop_after >= 2 else 0):
        s = g % 2
        norm_transpose_group((xt[s], Bxt[s], xsem[s], ssq[s], Bssq[s], xn, Bxn, junk, Bjunk),
                             xe[8 + g * 512:8 + (g + 1) * 512, :].rearrange("(t p) d -> p t d", p=128),
                             lambda c, g=g: hT[:, c, 8 + g * 512:8 + (g + 1) * 512], BhT, A_a, sh_a, (0, 1))
    xh = sb("xh", [16, D], F32, ph1)
    Bxh = Buf("xh")
    hsem = S.new_sem("halo")
    dma_in(hsem, xh[0:8, :], xe[0:8, :], [Bxh])
    dma_in(hsem, xh[8:16, :], xe[8 + NOWN:16 + NOWN, :], [Bxh])
    sqh = sb("sqh", [16, 2], F32, ph1)
    Bsqh = Buf("sqh")
    xnh = sb("xnh", [16, D], F32, ph1)
    act(junk[0:16, :], xh[:], AF.Square, [Bxh], [Bjunk, Bsqh], accum_out=sqh[:, 0:1])
    act(sqh[:, 1:2], sqh[:, 0:1], AF.Sqrt, [Bsqh, Bconst], [Bsqh], bias=epsc[0:16, 0:1], scale=1.0 / D)
    S.op("dve", lambda e: e.reciprocal(out=sqh[:, 1:2], in_=sqh[:, 1:2]), [Bsqh], [Bsqh])
    ts("dve", xnh[:], xh[:], sqh[:, 1:2], None, ALU.mult, None, [Bxh, Bsqh], [Bxh])

    def emit_h(e):
        ins = None
        for c in range(16):
            ins = e.transpose(pb[0][:, c * 16:(c + 1) * 16], xnh[:, c * 128:(c + 1) * 128], ident[0:16, 0:16])
        return ins
    S.op("pe", emit_h, [Bxh, Bconst], [BP[0]])
    htmp = sb("htmp", [128, 16, 16], F32, ph1)
    Bhtmp = Buf("htmp")
    for c in range(16):
        act(htmp[:, c, :], pb[0][:, c * 16:(c + 1) * 16], AF.Identity, [BP[0], Bmod], [Bhtmp],
            bias=sh_a[:, c:c + 1], scale=A_a[:, c:c + 1])
    ts("dve", hT[:, :, 0:8], htmp[:, :, 0:8], hflag[:, 0:1], None, ALU.mult, None, [Bhtmp, Bconst], [BhT])
    ts("dve", hT[:, :, 8 + NOWN:16 + NOWN], htmp[:, :, 8:16], hflag[:, 1:2], None, ALU.mult, None, [Bhtmp, Bconst], [BhT])
    if dbg:
        dma_in(st_sem, dbg_out["d_hT"], hT[:], [], reads=[BhT])
    S.barrier()
    ph1.close()

    if stop_after >= 3:
        ph3 = ExitStack()
        kTh = sb("kTh", [128, S_LEN], BF16, ph3)
        vh = sb("vh", [128, 64, 128], BF16, ph3)
        Bkv = Buf("kvh")
        kvsem = S.new_sem("kvld")
        cq = sb("cq", [128, 2, NOWN], F32, ph3)
        Bcq = Buf("cq")
        dma_in(ldc, cq[:, 0, :], cosq_d, [Bcq])
        dma_in(ldc, cq[:, 1, :], sinq_d, [Bcq])
        qT = sb("qT", [128, NOWN], BF16, ph3)
        BqT = Buf("qT")
        wsl = [sb(f"wsl{i}", [128, 16, 128], BF16, ph3) for i in range(3)]
        Bwsl = [Buf(f"wsl{i}") for i in range(3)]
        wslsem = [S.new_sem(f"wsl{i}") for i in range(3)]
        wslc = [0]

        def load_w(src):
            i = wslc[0] % 3
            wslc[0] += 1
            dma_in(wslsem[i], wsl[i][:], src, [Bwsl[i]], queue="pool")
            return wsl[i], Bwsl[i]
        pT = [sb(f"pT{i}", [128, 512], BF16, ph3) for i in range(4)]
        BpT = [Buf(f"pT{i}") for i in range(4)]
        wpl = sb("wpl", [128, 4, 512], BF16, ph3)
        Bwpl = Buf("wpl")
        wplsem = S.new_sem("wpl")
        pooled = sb("pooled", [128, 4, NOWN], BF16, ph3)
        Bpooled = Buf("pooled")
        mrgh = [sb(f"mrgh{i}", [128, NOWN], BF16, ph3) for i in range(2)]
        Bmrgh = [Buf(f"mrgh{i}") for i in range(2)]
        dacc = [sb(f"dacc{i}", [128, 512], F32, ph3) for i in range(2)]
        Bdacc = [Buf(f"dacc{i}") for i in range(2)]
        phB = None
        mst = S.new_sem("mst")
        Bmrgs = Buf("mrg_s")
        ATT_SCALE = 128.0 ** -0.5
        pti = 0
        for h in range(16):
            kvh, gi = h // 4, h // 4
            if h % 4 == 0:
                dma_in(kvsem, kTh[:], kT_s[kvh], [Bkv], reads=[Bks])
                dma_in(kvsem, vh[:].rearrange("p t d -> p (t d)"), v_s[kvh], [Bkv], reads=[Bvs])
                if phB is not None:
                    S.barrier()
                    phB.close()
                phA = ExitStack()
                pc = sb("pc", [128, NEXT], F32, phA)
                Bpc = Buf("pc")
                sA = sb("sA", [128, NEXT], F32, phA)
                sBt = sb("sBt", [128, NEXT], F32, phA)
                BsA = Buf("sA")
                BsB = Buf("sB")
                dma_in(wplsem, wpl[:], wpool_d[gi], [Bwpl], queue="pool")
                w = (2, 4, 8, 16)[gi]
                for cc in range(4):
                    wp_t, Bwp = load_w(wp_d[gi * 4 + cc])
                    col = 0
                    ci = 0
                    while col < NEXT:
                        n = min(512, NEXT - col)
                        bk = 6 + (ci % 2)
                        mm_group(pb[bk][:, 0:n], [(wp_t[:, kc, :], hT[:, kc, col:col + n]) for kc in range(16)],
                                 [Bwp, BhT], [BP[bk]])
                        act(pc[:, col:col + n], pb[bk][:, 0:n], AF.Copy, [BP[bk]], [Bpc])
                        col += n
                        ci += 1
                    tt("dve", sA[:, 1:NEXT], pc[:, 1:NEXT], pc[:, 0:NEXT - 1], ALU.add, [Bpc], [BsA])
                    cur, Bcur, oth, Both = sA, BsA, sBt, BsB
                    lo, hi = 1, NEXT
                    sh = 1
                    ww = 2
                    while ww < w:
                        nlo, nhi = lo + sh, hi - sh
                        tt("pool", oth[:, nlo:nhi], cur[:, nlo - sh:nhi - sh], cur[:, nlo + sh:nhi + sh], ALU.add,
                           [Bcur], [Both])
                        cur, Bcur, oth, Both = oth, Both, cur, Bcur
                        lo, hi = nlo, nhi
                        sh *= 2
                        ww *= 2
                    stt("dve", pooled[:, cc, :], cur[:, 8:8 + NOWN], 1.0 / w, pc[:, 8:8 + NOWN], ALU.mult, ALU.subtract,
                        [Bcur, Bpc], [Bpooled])
                    for (c0, e0) in ((0, 0), (NOWN - 8, 8)):
                        tt("dve", oth[:, 0:8], cur[:, 8 + c0:16 + c0], iedge[:, gi, e0:e0 + 8], ALU.mult,
                           [Bcur, Bconst], [Both])
                        tt("dve", pooled[:, cc, c0:c0 + 8], oth[:, 0:8], pc[:, 8 + c0:16 + c0], ALU.subtract,
                           [Both, Bpc], [Bpooled])
                S.barrier()
                phA.close()
                phB = ExitStack()
                qtmp = (sb("q_sq", [128, 512], BF16, phB), sb("q_tg", [128, 512], BF16, phB),
                        sb("q_u", [128, 512], F32, phB), sb("q_w", [128, 512], F32, phB), sb("q_rs", [128, 512], F32, phB))
                Bqtmp = [Buf(f"qtmp{i}") for i in range(5)]
                oT = sb("oT", [128, 512], F32, phB)
                BoT = Buf("oT")
                rden = sb("rden", [128, 512], F32, phB)
                Brden = Buf("rden")
                sga = sb("sga", [128, 512], F32, phB)
                sgp = sb("sgp", [128, 512], F32, phB)
                Bsga = Buf("sga")
                Bsgp = Buf("sgp")
            wq_t, Bwq = load_w(wq_d[h])
            wga_t, Bwga = load_w(wga_d[h])
            wgp_t, Bwgp = load_w(wgp_d[h])
            ms = h % 2
            for qg in range(4):
                bk = 6
                mm_group(pb[bk][:], [(wq_t[:, kc, :], hT[:, kc, 8 + qg * 512:8 + (qg + 1) * 512]) for kc in range(16)],
                         [Bwq, BhT], [BP[bk]])
                qk_post(bk, 0, cq[:, 0, qg * 512:(qg + 1) * 512], cq[:, 1, qg * 512:(qg + 1) * 512], Bcq,
                        qT[:, qg * 512:(qg + 1) * 512], BqT, qtmp, Bqtmp, 7, 6)
            for qg in range(4):
                ob, db = (2, 3) if qg % 2 == 0 else (4, 5)
                qs = qT[:, qg * 512:(qg + 1) * 512]
                sbanks = (0, 1, 7)

                def emit_s(kt, qs=qs):
                    sbk = sbanks[kt % 3]
                    mm_group(pb[sbk][:], [(kTh[:, kt * 128:(kt + 1) * 128], qs)], [Bkv, BqT], [BP[sbk]])
                emit_s(0)
                emit_s(1)
                for kt in range(64):
                    sbk = sbanks[kt % 3]
                    p_i = pti % 4
                    pti += 1
                    act(pT[p_i][:], pb[sbk][:], AF.Exp, [BP[sbk]], [BpT[p_i]], scale=ATT_SCALE)
                    if kt + 2 < 64:
                        emit_s(kt + 2)

                    def emit(e, kt=kt, p_i=p_i, ob=ob, db=db):
                        e.matmul(pb[ob][:], lhsT=vh[:, kt, :], rhs=pT[p_i][:], start=(kt == 0), stop=(kt == 63))
                        return e.matmul(pb[db][:], lhsT=onesb[:], rhs=pT[p_i][:], start=(kt == 0), stop=(kt == 63))
                    S.op("pe", emit, [Bkv, BpT[p_i], Bconst], [BP[ob], BP[db]])
                S.op("dve", lambda e, db=db, rden=rden: e.reciprocal(out=rden[:], in_=pb[db][:]), [BP[db]], [Brden])
                tt("dve", oT[:], pb[ob][:], rden[:], ALU.mult, [BP[ob], Brden], [BoT])
                cols = slice(8 + qg * 512, 8 + (qg + 1) * 512)
                oc = slice(qg * 512, (qg + 1) * 512)
                mm_group(pb[6][:], [(wga_t[:, kc, :], hT[:, kc, cols]) for kc in range(16)], [Bwga, BhT], [BP[6]])
                act(sga[:], pb[6][:], AF.Sigmoid, [BP[6]], [Bsga])
                mm_group(pb[7][:], [(wgp_t[:, kc, :], hT[:, kc, cols]) for kc in range(16)], [Bwgp, BhT], [BP[7]])
                act(sgp[:], pb[7][:], AF.Sigmoid, [BP[7]], [Bsgp])
                cc = h % 4
                mm_group(pb[6][:], [(wpl[:, kc, cc * 128:(cc + 1) * 128], pooled[:, kc, oc]) for kc in range(4)],
                         [Bwpl, Bpooled], [BP[6]])
                stt("dve", sgp[:], pb[6][:], pscT[:, h:h + 1], sgp[:], ALU.mult, ALU.mult, [BP[6], Bconst, Bsgp], [Bsgp])
                tt("pool", sga[:], sga[:], oT[:], ALU.mult, [Bsga, BoT], [Bsga])
                tt("dve", mrgh[ms][:, oc], sga[:], sgp[:], ALU.add, [Bsga, Bsgp], [Bmrgh[ms]])
            dma_in(mst, mrg_s[h], mrgh[ms][:], [Bmrgs], reads=[Bmrgh[ms]])
        if dbg:
            dma_in(st_sem, dbg_out["d_mrg"], mrg_s, [], reads=[Bmrgs])
        S.barrier()
        phB.close()
        ph3.close()
    ph23.close()
    es_hT_done = True

    if stop_after >= 4:
        ph4 = ExitStack()
        hT2 = sb("hT2", [128, 16, 1024], BF16, ph4)
        BhT2 = Buf("hT2")
        G = sb("G", [128, 8, NE + 1], F32, ph4)
        BG = Buf("G")
        wrt = sb("wrt", [128, 16, NE], F32, ph4)
        dma_in(ldc, wrt[:], wr_d, [Bconst])
        Bconst.w = (ldc, S.cnt[ldc])
        Bx1s = Buf("x1_s")
        x1st = S.new_sem("x1st")
        for half in range(2):
            p4 = ExitStack()
            wout = sb("wout", [128, 16, D], BF16, p4)
            Bwout = Buf("wout")
            wosem = S.new_sem(f"wo{half}")
            for q4 in range(4):
                dma_in(wosem, wout[:, q4 * 4:(q4 + 1) * 4, :], wout_d[:, q4 * 4:(q4 + 1) * 4, :], [Bwout], queue="pool")
            mt = [sb(f"mt{i}", [128, 16, 128], BF16, p4) for i in range(2)]
            Bmt = [Buf(f"mt{i}") for i in range(2)]
            mtsem = [S.new_sem(f"mt{half}{i}") for i in range(2)]
            xo = [sb(f"xo{i}", [128, D], F32, p4) for i in range(2)]
            Bxo = [Buf(f"xo{i}") for i in range(2)]
            xosem = [S.new_sem(f"xo{half}{i}") for i in range(2)]
            tmp = sb("tmp4", [128, D], F32, p4)
            Btmp = Buf("tmp4")
            x1t = sb("x1t", [128, D], F32, p4)
            Bx1t = Buf("x1t")
            xn2 = sb("xn2", [128, D], F32, p4)
            Bxn2 = Buf("xn2")
            junk4 = sb("junk4", [128, D], BF16, p4)
            Bjunk4 = Buf("junk4")
            h2f = sb("h2f", [128, 16, 128], F32, p4)
            Bh2f = Buf("h2f")
            st4 = sb("st4", [128, 16], F32, p4)
            Bst4 = Buf("st4")
            rt = {n: sb(f"rt_{n}", shp, F32, p4) for n, shp in
                  (("sc", [128, NE]), ("ch", [128, NE]), ("m8", [128, 8, 8]), ("gs", [128, 8]), ("g8", [128, 8]),
                   ("gm", [128, 8]), ("mch", [128, NE]), ("t8", [128, 8]), ("sel", [128, NE]), ("den", [128, 2]))}
            Brt = Buf("rt")
            def p4_loads(tl):
                tg2 = half * 8 + tl
                s2 = tl % 2
                dma_in(mtsem[s2], mt[s2][:], mrg_s[:, :, tg2 * 128:(tg2 + 1) * 128].rearrange("c p t -> p c t"),
                       [Bmt[s2]], reads=[Bmrgs] if stop_after >= 3 else [])
                dma_in(xosem[s2], xo[s2][:], xe[8 + tg2 * 128:8 + (tg2 + 1) * 128, :], [Bxo[s2]])
            p4_loads(0)
            for tl in range(8):
                tg_ = half * 8 + tl
                s = tl % 2
                if tl + 1 < 8:
                    p4_loads(tl + 1)
                for cg in range(4):
                    mm_group(pb[cg][:], [(mt[s][:, kc, :], wout[:, kc, cg * 512:(cg + 1) * 512]) for kc in range(16)],
                             [Bmt[s], Bwout], [BP[cg]])
                for cg in range(4):
                    act(tmp[:, cg * 512:(cg + 1) * 512], pb[cg][:], AF.Copy, [BP[cg]], [Btmp])
                act(junk4[:], tmp[:], AF.Square, [Btmp], [Bjunk4, Bst4], accum_out=st4[:, 4:5])
                rstd_from_ssq(st4[:, 4:5], st4[:, 5:6], Bst4, Bst4, 1.0 / D)
                stt("dve", tmp[:], tmp[:], st4[:, 5:6], gg[:, 0, :], ALU.mult, ALU.mult, [Btmp, Bst4, Bgg], [Btmp])
                tt("pool", x1t[:], tmp[:], xo[s][:], ALU.add, [Btmp, Bxo[s]], [Bx1t])
                dma_in(x1st, x1_s[tg_ * 128:(tg_ + 1) * 128, :], x1t[:], [Bx1s], reads=[Bx1t])
                act(junk4[:], x1t[:], AF.Square, [Bx1t], [Bjunk4, Bst4], accum_out=st4[:, 6:7])
                rstd_from_ssq(st4[:, 6:7], st4[:, 7:8], Bst4, Bst4, 1.0 / D)
                ts("dve", xn2[:], x1t[:], st4[:, 7:8], None, ALU.mult, None, [Bx1t, Bst4], [Bxn2])
                for q4 in range(4):
                    bk = 4 + q4

                    def emit(e, q4=q4, bk=bk):
                        ins = None
                        for j in range(4):
                            c = q4 * 4 + j
                            ins = e.transpose(pb[bk][:, j * 128:(j + 1) * 128], xn2[:, c * 128:(c + 1) * 128], ident[:])
                        return ins
                    S.op("pe", emit, [Bxn2, Bconst], [BP[bk]])
                    for j in range(4):
                        c = q4 * 4 + j
                        act(h2f[:, c, :], pb[bk][:, j * 128:(j + 1) * 128], AF.Identity, [BP[bk], Bmod], [Bh2f],
                            bias=sh_f[:, c:c + 1], scale=A_f[:, c:c + 1])
                cp("pool", hT2[:, :, tl * 128:(tl + 1) * 128], h2f[:], [Bh2f], [BhT2])
                mm_group(pb[0][:, 0:NE], [(h2f[:, c, :], wrt[:, c, :]) for c in range(16)], [Bh2f, Bconst], [BP[0]])
                act(rt["sc"][:], pb[0][:, 0:NE], AF.Sigmoid, [BP[0]], [Brt])
                tt("dve", rt["ch"][:], rt["sc"][:], rbias[:], ALU.add, [Brt, Bconst], [Brt])
                for g8 in range(8):
                    S.op("dve", lambda e, g8=g8: e.max(out=rt["m8"][:, g8, :], in_=rt["ch"][:, g8 * 8:(g8 + 1) * 8]),
                         [Brt], [Brt])
                tt("dve", rt["gs"][:], rt["m8"][:, :, 0], rt["m8"][:, :, 1], ALU.add, [Brt], [Brt])
                S.op("dve", lambda e: e.max(out=rt["g8"][:], in_=rt["gs"][:]), [Brt], [Brt])
                ts("dve", rt["gm"][:], rt["gs"][:], rt["g8"][:, 3:4], None, ALU.is_ge, None, [Brt], [Brt])
                ts("dve", rt["gm"][:], rt["gm"][:], 10.0, -10.0, ALU.mult, ALU.add, [Brt], [Brt])
                tt("dve", rt["mch"][:].rearrange("p (g e) -> p g e", g=8), rt["ch"][:].rearrange("p (g e) -> p g e", g=8),
                   rt["gm"][:].unsqueeze(2).to_broadcast([128, 8, 8]), ALU.add, [Brt], [Brt])
                S.op("dve", lambda e: e.max(out=rt["t8"][:], in_=rt["mch"][:]), [Brt], [Brt])
                ts("dve", rt["sel"][:], rt["mch"][:], rt["t8"][:, 5:6], None, ALU.is_ge, None, [Brt], [Brt])
                tt("dve", rt["sel"][:], rt["sel"][:], rt["sc"][:], ALU.mult, [Brt], [Brt])
                S.op("dve", lambda e: e.reduce_sum(out=rt["den"][:, 0:1], in_=rt["sel"][:], axis=AX.X), [Brt], [Brt])
                S.op("dve", lambda e: e.reciprocal(out=rt["den"][:, 1:2], in_=rt["den"][:, 0:1]), [Brt], [Brt])
                ts("dve", G[:, tl, 0:NE], rt["sel"][:], rt["den"][:, 1:2], 2.5, ALU.mult, ALU.mult, [Brt], [BG])
                S.op("dve", lambda e, tl=tl: e.memset(G[:, tl, NE:NE + 1], 1.0), [], [BG])
            if dbg:
                dma_in(st_sem, dbg_out["d_hT2"][:, :, half * 1024:(half + 1) * 1024], hT2[:], [], reads=[BhT2])
                dma_in(st_sem, dbg_out["d_G"][:, half * 8:(half + 1) * 8, :], G[:, :, 0:NE], [], reads=[BG])
            S.barrier()
            p4.close()
            if stop_after < 5:
                continue
            p5 = ExitStack()
            acc = sb("acc", [128, 8, D], F32, p5)
            Bacc = [Buf(f"acc{i}") for i in range(8)]
            p5a = ExitStack()
            NGU = 4
            wgr = [sb(f"wgr{i}", [128, 16, 128], BF16, p5a) for i in range(NGU)]
            wur = [sb(f"wur{i}", [128, 16, 128], BF16, p5a) for i in range(NGU)]
            Bgu = [Buf(f"gu{i}") for i in range(NGU)]
            gusem = [S.new_sem(f"gu{half}{i}") for i in range(NGU)]
            NDW = 3
            wdr = [sb(f"wdr{i}", [128, NF, 256], BF16, p5a) for i in range(NDW)]
            Bwd = [Buf(f"wd{i}") for i in range(NDW)]
            wdsem = [S.new_sem(f"wd{half}{i}") for i in range(NDW)]
            actT1 = sb("actT", [128, NF, 1024], BF16, p5a)
            actT = [actT1, actT1]
            BactT1 = Buf("actT")
            BactT = [BactT1, BactT1]
            slt = [sb(f"slt{i}", [128, 512], F32, p5a) for i in range(2)]
            Bslt = [Buf(f"slt{i}") for i in range(2)]
            gui = 0
            wdi = 0
            pbi = 0
            sli = 0
            for e_ in range(NE + 1):
                a_i = e_ % 2
                for f in range(NF):
                    r = gui % NGU
                    gui += 1
                    dma_in(gusem[r], wgr[r][:], weg_d[e_, f], [Bgu[r]], queue="pool")
                    dma_in(gusem[r], wur[r][:], weu_d[e_, f], [Bgu[r]], queue="pool")
                    for tg_ in range(2):
                        gb = (pbi % 3) * 2
                        pbi += 1
                        ub = gb + 1
                        tk = slice(tg_ * 512, (tg_ + 1) * 512)
                        mm_group(pb[gb][:], [(wgr[r][:, kc, :], hT2[:, kc, tk]) for kc in range(16)],
                                 [Bgu[r], BhT2], [BP[gb]])
                        mm_group(pb[ub][:], [(wur[r][:, kc, :], hT2[:, kc, tk]) for kc in range(16)],
                                 [Bgu[r], BhT2], [BP[ub]])
                        si = sli % 2
                        sli += 1
                        act(slt[si][:], pb[gb][:], AF.Silu, [BP[gb]], [Bslt[si]])
                        tt("dve", actT[a_i][:, f, tk], slt[si][:], pb[ub][:], ALU.mult, [Bslt[si], BP[ub]], [BactT[a_i]])
                for c8 in range(8):
                    r = wdi % NDW
                    wdi += 1
                    dma_in(wdsem[r], wdr[r][:], wed_d[e_, c8], [Bwd[r]], queue="pool")
                    for tl in range(8):
                        bk = 6 + (tl % 2)
                        mm_group(pb[bk][:, 0:256], [(actT[a_i][:, f, tl * 128:(tl + 1) * 128], wdr[r][:, f, :])
                                                    for f in range(NF)], [BactT[a_i], Bwd[r]], [BP[bk]])
                        dst = acc[:, tl, c8 * 256:(c8 + 1) * 256]
                        if e_ == 0:
                            ts("dve", dst, pb[bk][:, 0:256], G[:, tl, e_:e_ + 1], None, ALU.mult, None,
                               [BP[bk], BG], [Bacc[tl]])
                        else:
                            stt("dve", dst, pb[bk][:, 0:256], G[:, tl, e_:e_ + 1], dst, ALU.mult, ALU.add,
                                [BP[bk], BG, Bacc[tl]], [Bacc[tl]])
            S.barrier()
            p5a.close()
            x1r = [sb(f"x1r{i}", [128, D], F32, p5) for i in range(2)]
            Bx1r = [Buf(f"x1r{i}") for i in range(2)]
            x1sem = [S.new_sem(f"x1r{half}{i}") for i in range(2)]
            fo = [sb(f"fo{i}", [128, D], F32, p5) for i in range(2)]
            Bfo = [Buf(f"fo{i}") for i in range(2)]
            junk5 = sb("junk5", [128, D], BF16, p5)
            Bjunk5 = Buf("junk5")
            st5 = sb("st5", [128, 16], F32, p5)
            Bst5 = Buf("st5")
            def f_loads(tl):
                tg2 = half * 8 + tl
                s2 = tl % 2
                dma_in(x1sem[s2], x1r[s2][:], x1_s[tg2 * 128:(tg2 + 1) * 128, :], [Bx1r[s2]], reads=[Bx1s])
            f_loads(0)
            for tl in range(8):
                tg_ = half * 8 + tl
                s = tl % 2
                if tl + 1 < 8:
                    f_loads(tl + 1)
                act(junk5[:], acc[:, tl, :], AF.Square, [Bacc[tl]], [Bjunk5, Bst5], accum_out=st5[:, 2 * s:2 * s + 1])
                rstd_from_ssq(st5[:, 2 * s:2 * s + 1], st5[:, 2 * s + 1:2 * s + 2], Bst5, Bst5, 1.0 / D)
                stt("dve", fo[s][:], acc[:, tl, :], st5[:, 2 * s + 1:2 * s + 2], gg[:, 1, :], ALU.mult, ALU.mult,
                    [Bacc[tl], Bst5, Bgg], [Bfo[s]])
                tt("pool", fo[s][:], fo[s][:], x1r[s][:], ALU.add, [Bfo[s], Bx1r[s]], [Bfo[s]])
                dma_in(st_sem, out_d[tg_ * 128:(tg_ + 1) * 128, :], fo[s][:], [], reads=[Bfo[s]])
            S.barrier()
            p5.close()
        if dbg:
            dma_in(st_sem, dbg_out["d_x1"], x1_s, [], reads=[Bx1s])
        ph4.close()

    S.barrier()

    block = ec(nc.Block())

    @block.sync
    def _(e):
        S.replay("sp", e)

    @block.scalar
    def _(e):
        S.replay("act", e)

    @block.vector
    def _(e):
        S.replay("dve", e)

    @block.gpsimd
    def _(e):
        S.replay("pool", e)

    @block.tensor
    def _(e):
        S.replay("pe", e)

    es.close()
    return nc


def _rope_tables():
    rows = S_LEN // 64
    t = np.arange(S_LEN)
    row = (t // 64).astype(np.float32)
    col = (t % 64).astype(np.float32)
    inv_freq = (np.float32(10000.0) ** (-np.arange(0, 64, 2, dtype=np.float32) / np.float32(64))).astype(np.float32)
    cosT = np.zeros((128, S_LEN), np.float32)
    sinT = np.zeros((128, S_LEN), np.float32)
    perm = np.zeros((128, 128), np.float32)
    for d in range(128):
        axis, r = d // 64, d % 64
        half, i = r // 32, r % 32
        pos = row if axis == 0 else col
        ang = (pos * inv_freq[i]).astype(np.float32)
        cosT[d] = np.cos(ang)
        sinT[d] = -np.sin(ang) if half == 0 else np.sin(ang)
        partner = d + 32 if half == 0 else d - 32
        perm[partner, d] = 1.0
    return cosT, sinT, perm


def _ktile(w, nk=16):
    K, M = w.shape
    return np.ascontiguousarray(w.reshape(K // 128, 128, M).transpose(1, 0, 2))


def _prep_shared(inp, with_experts=True):
    f = np.float32
    w_in = np.asarray(inp["w_in"], f)[0]
    sh = {}
    sh["wmod_l"] = np.ascontiguousarray(np.asarray(inp["w_mod"], f)[0].reshape(16, 128, 24, 512).transpose(2, 1, 0, 3))
    b_mod = np.asarray(inp["b_mod"], f)[0]
    sh["bmodT"] = np.ascontiguousarray(b_mod.reshape(96, 128).T)
    sh["bgt_rep"] = np.ascontiguousarray(np.broadcast_to(
        np.stack([b_mod[4096:6144], b_mod[10240:12288]])[None], (128, 2, D)))
    sh["gpost_rep"] = np.ascontiguousarray(np.broadcast_to(
        np.stack([np.asarray(inp["g_post_mix"], f)[0], np.asarray(inp["g_post_ffn"], f)[0]])[None], (128, 2, D)))
    sh["gpreT"] = np.ascontiguousarray(np.stack([np.asarray(inp["g_pre_mix"], f)[0].reshape(16, 128).T,
                                                 np.asarray(inp["g_pre_ffn"], f)[0].reshape(16, 128).T], axis=1))
    sh["qkg"] = np.ascontiguousarray(np.stack([np.asarray(inp["q_norm_g"], f)[0], np.asarray(inp["k_norm_g"], f)[0]], axis=1))
    sh["wk_l"] = _ktile(w_in[:, 2048:2560])
    sh["wv_l"] = _ktile(w_in[:, 2560:3072])

    def heads(wc):
        return np.ascontiguousarray(wc.reshape(16, 128, 16, 128).transpose(2, 1, 0, 3))
    sh["wq_l"] = heads(w_in[:, 0:2048])
    sh["wp_l"] = heads(w_in[:, 3072:5120])
    sh["wga_l"] = heads(w_in[:, 5120:7168])
    sh["wgp_l"] = heads(w_in[:, 7168:9216])
    sh["wpool_l"] = np.ascontiguousarray(np.asarray(inp["w_pool"], f)[0].reshape(4, 4, 128, 512).transpose(0, 2, 1, 3))
    sh["pscT"] = np.ascontiguousarray(np.asarray(inp["pool_scale"], f)[0].reshape(16, 128).T)
    sh["wout_l"] = _ktile(np.asarray(inp["w_out"], f)[0])
    sh["wr_l"] = _ktile(np.asarray(inp["w_router"], f)[0])
    sh["rbias_rep"] = np.ascontiguousarray(np.broadcast_to(np.asarray(inp["router_bias"], f)[0][None], (128, NE)))

    sh["ident"] = np.eye(128, dtype=f)
    cosT, sinT, perm = _rope_tables()
    sh["perm"] = perm
    sh["cosk"] = cosT
    sh["sink"] = sinT

    def gu(we, ws):
        w = np.concatenate([np.asarray(we, f)[0], np.asarray(ws, f)[0][None]], axis=0)
        return np.ascontiguousarray(w.reshape(NE + 1, 16, 128, NF, 128).transpose(0, 3, 2, 1, 4))
    if not with_experts:
        return sh, cosT, sinT
    sh["weg_l"] = gu(inp["w_exp_gate"], inp["w_sh_gate"])
    sh["weu_l"] = gu(inp["w_exp_up"], inp["w_sh_up"])
    wd = np.concatenate([np.asarray(inp["w_exp_down"], f)[0], np.asarray(inp["w_sh_down"], f)[0][None]], axis=0)
    sh["wed_l"] = np.ascontiguousarray(wd.reshape(NE + 1, NF, 128, 8, 256).transpose(0, 3, 2, 1, 4))
    return sh, cosT, sinT


def _prep_core(inp, sh, cosT, sinT, core):
    f = np.float32
    x = np.asarray(inp["x"], f)
    c = np.asarray(inp["c"], f)
    b, pos = core // 4, core % 4
    t0 = pos * NOWN
    m = dict(sh)
    m["xb"] = x[b]
    xe = np.zeros((NEXT, D), f)
    xe[8:8 + NOWN] = x[b, t0:t0 + NOWN]
    if pos > 0:
        xe[0:8] = x[b, t0 - 8:t0]
    if pos < 3:
        xe[8 + NOWN:] = x[b, t0 + NOWN:t0 + NOWN + 8]
    m["xe"] = xe
    m["cT"] = np.ascontiguousarray(c[b].reshape(16, 128).T)
    m["cosq"] = np.ascontiguousarray(cosT[:, t0:t0 + NOWN])
    m["sinq"] = np.ascontiguousarray(sinT[:, t0:t0 + NOWN])
    m["hflag"] = np.ascontiguousarray(np.broadcast_to(np.array([pos > 0, pos < 3], f)[None], (128, 2)))
    ie = np.zeros((4, 16), f)
    for wi, w in enumerate((2, 4, 8, 16)):
        for j in range(16):
            t = t0 + j if j < 8 else t0 + NOWN - 8 + (j - 8)
            lo = max(t - w // 2, 0)
            hi = min(t - w // 2 + w, S_LEN)
            ie[wi, j] = 1.0 / float(hi - lo)
    m["iedge"] = np.ascontiguousarray(np.broadcast_to(ie[None], (128, 4, 16)))
    return m


def kernel(**inputs):
    import time
    _t0 = time.time()
    stop_after = DEBUG.get("stop_after", 99)
    sh, cosT, sinT = _prep_shared(inputs, with_experts=stop_after >= 5)
    in_maps = [_prep_core(inputs, sh, cosT, sinT, core) for core in range(8)]
    dbg = DEBUG.get("dbg", False)
    _t1 = time.time()
    nc = build_program(stop_after=stop_after, dbg=dbg)
    _t2 = time.time()
    ncores = DEBUG.get("ncores", 8)
    if DEBUG.get("trace"):
        res = run_bass_kernel_spmd(nc, in_maps[:ncores], core_ids=list(range(ncores)), trace=True)
        print("[kernel] exec_time_ns", res.exec_time_ns, flush=True)
    else:
        res = run_bass_kernel_spmd(nc, in_maps[:ncores], core_ids=list(range(ncores)))
    print(f"[kernel] prep {_t1 - _t0:.1f}s build {_t2 - _t1:.1f}s run {time.time() - _t2:.1f}s", flush=True)
    DEBUG["res"] = res
    out = np.zeros((2, S_LEN, D), np.float32)
    for core in range(ncores):
        b, pos = core // 4, core % 4
        out[b, pos * NOWN:(pos + 1) * NOWN] = np.asarray(res.results[core]["out"], np.float32)
    return out
```
